# Optimizing a Trainium2 kernel written in Bass

```python
import math
import jax, jax.numpy as jnp
from jax import lax
import numpy as np

D_MODEL = 1024
BATCH = 2
SEQ = 16384
DEPTH = 2

N_BRANCHES = 4
BRANCH_WIDTH = D_MODEL // N_BRANCHES
DIFF_HEADS = 4
DIFF_V_DIM = BRANCH_WIDTH // DIFF_HEADS
DIFF_QK_DIM = DIFF_V_DIM // 2
Q_BLOCK = 128
POOL_WINDOWS = (2, 4, 8, 16)
POOL_GROUP_WIDTH = BRANCH_WIDTH // 4
SCONV_WIDTH = 3
DELTA_HEADS = 4
DELTA_DK = BRANCH_WIDTH // DELTA_HEADS
DELTA_DV = BRANCH_WIDTH // DELTA_HEADS
DELTA_CONV_WIDTH = 5
DELTA_CHUNK = 64
D_FF = 2816
N_EXPERTS = 8
TOP_K = 2
D_FF_EXPERT = 2816
MOE_BLOCK = 128
N_ADA = 6
EPS = 1e-6

IN_SPLITS = (
    DIFF_HEADS * DIFF_QK_DIM, DIFF_HEADS * DIFF_QK_DIM,
    DIFF_HEADS * DIFF_QK_DIM, DIFF_HEADS * DIFF_QK_DIM,
    DIFF_HEADS * DIFF_V_DIM,
    BRANCH_WIDTH,
    BRANCH_WIDTH, BRANCH_WIDTH, BRANCH_WIDTH,
    DELTA_HEADS * DELTA_DK, DELTA_HEADS * DELTA_DK,
    DELTA_HEADS * DELTA_DV, DELTA_HEADS * DELTA_DV,
    2 * DELTA_HEADS, 2 * DELTA_HEADS,
)
IN_COLS = sum(IN_SPLITS)

kernel_name = "hybrid_bidir_diffattn_pool_conv_deltanet_moe"

F32 = jnp.float32


def rms_norm(x, g):
    xf = x.astype(F32)
    y = xf * lax.rsqrt(jnp.mean(xf * xf, axis=-1, keepdims=True) + EPS)
    return (y * g.astype(F32)).astype(x.dtype)


def l2_normalize(x):
    xf = x.astype(F32)
    return (xf * lax.rsqrt(jnp.sum(xf * xf, axis=-1, keepdims=True) + EPS)).astype(x.dtype)


def centred_depthwise_conv(z, w):
    k = w.shape[0]
    r = k // 2
    s = z.shape[1]
    zp = jnp.pad(z, ((0, 0), (r, r), (0, 0)))
    out = zp[:, 0:s] * w[0]
    for j in range(1, k):
        out = out + zp[:, j:j + s] * w[j]
    return out


def alibi_slopes():
    return jnp.array([2.0 ** (-8.0 * (i + 1) / DIFF_HEADS) for i in range(DIFF_HEADS)], F32)


def diff_attention(q1, q2, k1, k2, v, lam):
    b_, s, h, dv = v.shape
    scale = DIFF_QK_DIM ** -0.5
    slopes = alibi_slopes()
    kpos = jnp.arange(s)

    def block(i):
        start = i * Q_BLOCK
        qa = lax.dynamic_slice_in_dim(q1, start, Q_BLOCK, axis=1)
        qb = lax.dynamic_slice_in_dim(q2, start, Q_BLOCK, axis=1)
        qpos = start + jnp.arange(Q_BLOCK)
        bias = -slopes[:, None, None] * jnp.abs(qpos[:, None] - kpos[None, :]).astype(F32)
        s1 = jnp.einsum('bqhd,bkhd->bhqk', qa, k1).astype(F32) * scale + bias
        s2 = jnp.einsum('bqhd,bkhd->bhqk', qb, k2).astype(F32) * scale + bias
        a = jax.nn.softmax(s1, axis=-1) - lam * jax.nn.softmax(s2, axis=-1)
        return jnp.einsum('bhqk,bkhd->bqhd', a.astype(v.dtype), v)

    out = lax.map(block, jnp.arange(s // Q_BLOCK))
    return jnp.moveaxis(out, 0, 1).reshape(b_, s, h, dv)


def multiscale_pool(p, pool_w, pool_scale):
    b_, s, _ = p.shape
    pf = p.astype(F32)
    cs = jnp.concatenate([jnp.zeros((b_, 1, BRANCH_WIDTH), F32), jnp.cumsum(pf, axis=1)], axis=1)
    t = jnp.arange(s)
    outs = []
    for gi, w in enumerate(POOL_WINDOWS):
        lo = jnp.clip(t - w // 2, 0, s)
        hi = jnp.clip(t + w // 2, 0, s)
        sl = slice(gi * POOL_GROUP_WIDTH, (gi + 1) * POOL_GROUP_WIDTH)
        csg = cs[..., sl]
        mean = (csg[:, hi] - csg[:, lo]) / (hi - lo).astype(F32)[None, :, None]
        outs.append(mean - pf[..., sl])
    m = jnp.stack(outs, axis=2)
    y = jnp.einsum('bsgc,gce->bsge', m, pool_w.astype(F32)).reshape(b_, s, BRANCH_WIDTH)
    return (y * pool_scale.astype(F32)).astype(p.dtype)


def gated_delta_rule(q, k, v, g, beta):
    b_, h, l, dk = q.shape
    dv = v.shape[-1]
    c = DELTA_CHUNK
    n = l // c
    q, k, v = (t.astype(F32).reshape(b_, h, n, c, -1) for t in (q, k, v))
    g = jnp.cumsum(g.astype(F32).reshape(b_, h, n, c), axis=-1)
    beta = beta.astype(F32).reshape(b_, h, n, c)
    lower = jnp.tril(jnp.ones((c, c), bool))
    strict = jnp.tril(jnp.ones((c, c), bool), -1)
    gdiff = g[..., :, None] - g[..., None, :]
    decay = jnp.where(lower, jnp.exp(jnp.where(lower, gdiff, 0.0)), 0.0)
    k_beta = k * beta[..., None]
    m = jnp.where(strict, jnp.einsum('bhnid,bhnjd->bhnij', k_beta, k) * decay, 0.0)
    eye = jnp.eye(c, dtype=F32)
    t_inv = lax.linalg.triangular_solve(eye + m, jnp.broadcast_to(eye, m.shape),
                                        left_side=True, lower=True)
    u = t_inv @ (v * beta[..., None])
    w = t_inv @ (k_beta * jnp.exp(g)[..., None])
    attn = jnp.einsum('bhnid,bhnjd->bhnij', q, k) * decay
    q_g = q * jnp.exp(g)[..., None]
    g_last = g[..., -1]
    k_d = k * jnp.exp(g_last[..., None] - g)[..., None]

    def step(state, xs):
        u_n, w_n, attn_n, qg_n, kd_n, gl_n = xs
        v_new = u_n - w_n @ state
        o = qg_n @ state + attn_n @ v_new
        state = state * jnp.exp(gl_n)[..., None, None] + jnp.swapaxes(kd_n, -1, -2) @ v_new
        return state, o

    xs = tuple(jnp.moveaxis(t, 2, 0) for t in (u, w, attn, q_g, k_d, g_last))
    _, o = lax.scan(step, jnp.zeros((b_, h, dk, dv), F32), xs)
    return jnp.moveaxis(o, 0, 2).reshape(b_, h, l, dv)


def token_mixer(h, w_in, diff_lambda, diff_subln, pool_w, pool_scale, sconv_w,
                delta_conv_w, delta_a_log, delta_dt_bias, delta_norm,
                w_branch, w_merge, b_merge, w_o, lambda_init):
    b_, s, _ = h.shape
    proj = h @ w_in
    cuts = np.cumsum(IN_SPLITS)[:-1].tolist()
    (q1, q2, k1, k2, va, pin, cb, cc, cx,
     dq, dk, dv, dz, dbeta, da) = jnp.split(proj, cuts, axis=-1)

    def heads(t, d):
        return t.reshape(b_, s, -1, d)

    lq1, lk1, lq2, lk2 = (diff_lambda[i].astype(F32) for i in range(4))
    lam = jnp.exp(jnp.sum(lq1 * lk1)) - jnp.exp(jnp.sum(lq2 * lk2)) + lambda_init
    oa = diff_attention(heads(q1, DIFF_QK_DIM), heads(q2, DIFF_QK_DIM),
                        heads(k1, DIFF_QK_DIM), heads(k2, DIFF_QK_DIM),
                        heads(va, DIFF_V_DIM), lam)
    oa = (rms_norm(oa, diff_subln) * (1.0 - lambda_init)).reshape(b_, s, BRANCH_WIDTH)

    ob = multiscale_pool(pin, pool_w, pool_scale)

    oc = cb * centred_depthwise_conv(cc * cx, sconv_w)

    qkv = jax.nn.silu(centred_depthwise_conv(jnp.concatenate([dq, dk, dv], axis=-1), delta_conv_w))
    dq, dk, dv = jnp.split(qkv, [DELTA_HEADS * DELTA_DK, 2 * DELTA_HEADS * DELTA_DK], axis=-1)
    q = l2_normalize(heads(dq, DELTA_DK)) * (DELTA_DK ** -0.5)
    k = l2_normalize(heads(dk, DELTA_DK))
    v = heads(dv, DELTA_DV)
    beta = jax.nn.sigmoid(dbeta.astype(F32)).reshape(b_, s, 2, DELTA_HEADS)
    g = -jnp.exp(delta_a_log.astype(F32)) * jax.nn.softplus(
        da.astype(F32).reshape(b_, s, 2, DELTA_HEADS) + delta_dt_bias.astype(F32))
    qh, kh, vh = (jnp.swapaxes(t, 1, 2) for t in (q, k, v))
    g_f, g_b = jnp.swapaxes(g[:, :, 0], 1, 2), jnp.swapaxes(g[:, :, 1], 1, 2)
    be_f, be_b = jnp.swapaxes(beta[:, :, 0], 1, 2), jnp.swapaxes(beta[:, :, 1], 1, 2)

    def flip(t):
        return jnp.flip(t, axis=2)

    fwd = gated_delta_rule(qh, kh, vh, g_f, be_f)
    bwd = flip(gated_delta_rule(flip(qh), flip(kh), flip(vh), flip(g_b), flip(be_b)))
    od = jnp.swapaxes(fwd + bwd, 1, 2).astype(h.dtype)
    od = (rms_norm(od, delta_norm) * jax.nn.silu(heads(dz, DELTA_DV))).reshape(b_, s, BRANCH_WIDTH)

    merged = None
    for i, o in enumerate((oa, ob, oc, od)):
        gate = jax.nn.sigmoid(h @ w_merge[i] + b_merge[i])
        term = gate * (o.astype(h.dtype) @ w_branch[i])
        merged = term if merged is None else merged + term
    return merged @ w_o


def swiglu(h, w_gate, w_up, w_down):
    return (jax.nn.silu(h @ w_gate) * (h @ w_up)) @ w_down


def moe_swiglu(h, router_w, router_b, w_gate, w_up, w_down):
    b_, s, d = h.shape
    xt = h.reshape(-1, d)
    t = xt.shape[0]
    logits = (xt @ router_w).astype(F32) + router_b.astype(F32)
    probs = jax.nn.softmax(logits, axis=-1)
    top_p, top_e = lax.top_k(probs, TOP_K)
    top_p = top_p / jnp.sum(top_p, axis=-1, keepdims=True)
    a = t * TOP_K
    e_flat = top_e.reshape(-1)
    p_flat = top_p.reshape(-1)
    tok = jnp.arange(a) // TOP_K
    order = jnp.argsort(e_flat)
    e_sorted = e_flat[order]
    counts = jnp.bincount(e_flat, length=N_EXPERTS)
    padded = ((counts + MOE_BLOCK - 1) // MOE_BLOCK) * MOE_BLOCK
    start = jnp.cumsum(counts) - counts
    pend = jnp.cumsum(padded)
    pstart = pend - padded
    dest = pstart[e_sorted] + (jnp.arange(a) - start[e_sorted])
    n_slots = ((a + MOE_BLOCK - 1) // MOE_BLOCK + N_EXPERTS) * MOE_BLOCK
    slot_tok = jnp.zeros((n_slots,), jnp.int32).at[dest].set(tok[order].astype(jnp.int32))
    slot_p = jnp.zeros((n_slots,), F32).at[dest].set(p_flat[order])
    nb = n_slots // MOE_BLOCK
    blk_e = jnp.minimum(jnp.searchsorted(pend, jnp.arange(nb) * MOE_BLOCK, side='right'),
                        N_EXPERTS - 1)

    def run_block(args):
        tk, e = args
        xb = xt[tk]
        return swiglu(xb, w_gate[e], w_up[e], w_down[e])

    y = lax.map(run_block, (slot_tok.reshape(nb, MOE_BLOCK), blk_e)).reshape(n_slots, d)
    y = y * slot_p[:, None].astype(y.dtype)
    out = jnp.zeros_like(xt).at[slot_tok].add(y)
    return out.reshape(b_, s, d)


def setup_inputs(seed: int = 0) -> dict:
    key = jax.random.key(seed)
    ks = iter(jax.random.split(key, 40))
    L = DEPTH
    ND = (DEPTH + 1) // 2
    NM = DEPTH // 2

    def nrm(shape, scale):
        return jax.random.normal(next(ks), shape, F32) * scale

    def gain(shape):
        return 1.0 + nrm(shape, 0.02)

    dt = jnp.exp(jax.random.uniform(next(ks), (L, 2, DELTA_HEADS), F32,
                                    math.log(1e-3), math.log(1e-1)))
    inputs = {
        "x": nrm((BATCH, SEQ, D_MODEL), 1.0),
        "c": nrm((BATCH, D_MODEL), 1.0),
        "ada_w": nrm((L, D_MODEL, N_ADA * D_MODEL), 0.5 * D_MODEL ** -0.5),
        "ada_b": nrm((L, N_ADA * D_MODEL), 0.02),
        "norm_mix_pre": gain((L, D_MODEL)),
        "norm_mix_post": gain((L, D_MODEL)),
        "norm_ffn_pre": gain((L, D_MODEL)),
        "norm_ffn_post": gain((L, D_MODEL)),
        "w_in": nrm((L, D_MODEL, IN_COLS), D_MODEL ** -0.5),
        "diff_lambda": nrm((L, 4, DIFF_QK_DIM), 0.1),
        "diff_subln": gain((L, DIFF_V_DIM)),
        "pool_w": nrm((L, 4, POOL_GROUP_WIDTH, POOL_GROUP_WIDTH), POOL_GROUP_WIDTH ** -0.5),
        "pool_scale": 1.0 + nrm((L, BRANCH_WIDTH), 0.1),
        "sconv_w": nrm((L, SCONV_WIDTH, BRANCH_WIDTH), SCONV_WIDTH ** -0.5),
        "delta_conv_w": nrm((L, DELTA_CONV_WIDTH, DELTA_HEADS * (2 * DELTA_DK + DELTA_DV)),
                            DELTA_CONV_WIDTH ** -0.5),
        "delta_a_log": jnp.log(jax.random.uniform(next(ks), (L, 2, DELTA_HEADS), F32, 1.0, 16.0)),
        "delta_dt_bias": dt + jnp.log(-jnp.expm1(-dt)),
        "delta_norm": gain((L, DELTA_DV)),
        "w_branch": nrm((L, N_BRANCHES, BRANCH_WIDTH, D_MODEL), BRANCH_WIDTH ** -0.5),
        "w_merge": nrm((L, N_BRANCHES, D_MODEL, D_MODEL), D_MODEL ** -0.5),
        "b_merge": nrm((L, N_BRANCHES, D_MODEL), 0.02),
        "w_o": nrm((L, D_MODEL, D_MODEL), D_MODEL ** -0.5),
        "ffn_w_gate": nrm((ND, D_MODEL, D_FF), D_MODEL ** -0.5),
        "ffn_w_up": nrm((ND, D_MODEL, D_FF), D_MODEL ** -0.5),
        "ffn_w_down": nrm((ND, D_FF, D_MODEL), D_FF ** -0.5),
        "router_w": nrm((NM, D_MODEL, N_EXPERTS), D_MODEL ** -0.5),
        "router_b": nrm((NM, N_EXPERTS), 0.01),
        "moe_w_gate": nrm((NM, N_EXPERTS, D_MODEL, D_FF_EXPERT), D_MODEL ** -0.5),
        "moe_w_up": nrm((NM, N_EXPERTS, D_MODEL, D_FF_EXPERT), D_MODEL ** -0.5),
        "moe_w_down": nrm((NM, N_EXPERTS, D_FF_EXPERT, D_MODEL), D_FF_EXPERT ** -0.5),
    }
    return inputs


def reference(x, c, ada_w, ada_b, norm_mix_pre, norm_mix_post, norm_ffn_pre, norm_ffn_post,
              w_in, diff_lambda, diff_subln, pool_w, pool_scale, sconv_w, delta_conv_w,
              delta_a_log, delta_dt_bias, delta_norm, w_branch, w_merge, b_merge, w_o,
              ffn_w_gate, ffn_w_up, ffn_w_down, router_w, router_b,
              moe_w_gate, moe_w_up, moe_w_down):
    cond = jax.nn.silu(c)
    for layer in range(DEPTH):
        mod = cond @ ada_w[layer] + ada_b[layer]
        sh1, sc1, g1, sh2, sc2, g2 = jnp.split(mod[:, None, :], N_ADA, axis=-1)
        lambda_init = 0.8 - 0.6 * math.exp(-0.3 * layer)

        h = rms_norm(x, norm_mix_pre[layer]) * (1.0 + sc1) + sh1
        f = token_mixer(h, w_in[layer], diff_lambda[layer], diff_subln[layer], pool_w[layer],
                        pool_scale[layer], sconv_w[layer], delta_conv_w[layer],
                        delta_a_log[layer], delta_dt_bias[layer], delta_norm[layer],
                        w_branch[layer], w_merge[layer], b_merge[layer], w_o[layer], lambda_init)
        x = x + g1 * rms_norm(f, norm_mix_post[layer])

        h = rms_norm(x, norm_ffn_pre[layer]) * (1.0 + sc2) + sh2
        if layer % 2 == 0:
            j = layer // 2
            f = swiglu(h, ffn_w_gate[j], ffn_w_up[j], ffn_w_down[j])
        else:
            j = layer // 2
            f = moe_swiglu(h, router_w[j], router_b[j], moe_w_gate[j], moe_w_up[j], moe_w_down[j])
        x = x + g2 * rms_norm(f, norm_ffn_post[layer])
    return x
```

```python
import os
from contextlib import ExitStack
import numpy as np
import ml_dtypes
import concourse.bass as bass
import concourse.mybir as mybir
from concourse.bass_utils import run_bass_kernel_spmd

F32 = mybir.dt.float32
BF16 = mybir.dt.bfloat16
I32 = mybir.dt.int32
AF = mybir.ActivationFunctionType
ALU = mybir.AluOpType
AX = mybir.AxisListType

SEM_LIM = 30000
N_DMA_SEMS = 8


class Buf:
    __slots__ = ("name", "last_w", "readers", "psum")

    def __init__(self, name="", psum=False):
        self.name = name
        self.psum = psum
        self.last_w = None
        self.readers = []


class Op:
    __slots__ = ("eng", "fn", "deps", "dma", "signal", "epoch", "idx", "gidx", "dsem", "dval")

    def __init__(self, eng, fn, dma):
        self.eng = eng
        self.fn = fn
        self.dma = dma
        self.deps = []
        self.signal = False
        self.epoch = 0
        self.idx = 0
        self.dsem = None
        self.dval = 0


class Prog:
    ENGS = ("sync", "scalar", "vector", "gpsimd", "tensor")

    _count = [0]

    def __init__(self, nc, same_engine_sync=True):
        Prog._count[0] += 1
        self.pid = Prog._count[0]
        self.nc = nc
        self.ops = []
        self.same_engine_sync = same_engine_sync
        self.stack = ExitStack()

    def rank_val(self, e, s, ename, mul=1):
        cache = getattr(self.nc, "_rank_cache", None)
        if cache is None:
            cache = {}
            setattr(self.nc, "_rank_cache", cache)
        key = (ename, s, mul)
        if key not in cache:
            v = (e.partition_id() + (s + 4)) % 4
            if mul != 1:
                v = v * mul
            cache[key] = e.snap(v)
        return cache[key]

    def sbuf(self, name, shape, dt):
        return self.stack.enter_context(self.nc.sbuf_tensor(f"sb{self.pid}_" + name, list(shape), dt))

    def psum(self, name, shape, dt=F32):
        return self.stack.enter_context(self.nc.psum_tensor(f"pp{self.pid}_" + name, list(shape), dt))

    def dram(self, name, shape, dt, kind="Internal"):
        return self.nc.dram_tensor(f"dr{self.pid}_" + name, list(shape), dt, kind=kind)

    def op(self, eng, fn, reads=(), writes=(), dma=False):
        o = Op(eng, fn, dma)
        o.gidx = len(self.ops)
        deps = {}
        for b in reads:
            if b.last_w is not None:
                deps[id(b.last_w)] = b.last_w
            if b.psum:
                for r in b.readers:
                    if r.eng != eng:
                        deps[id(r)] = r
        for b in writes:
            if b.last_w is not None:
                deps[id(b.last_w)] = b.last_w
            for r in b.readers:
                deps[id(r)] = r
        deps.pop(id(o), None)
        for b in reads:
            if not dma:
                b.readers = [r for r in b.readers if r.dma or r.eng != eng]
            b.readers.append(o)
        for b in writes:
            b.last_w = o
            b.readers = []
        dl = []
        latest = {}
        for d in deps.values():
            if (not d.dma) and (not dma) and d.eng == eng:
                if eng == "tensor" or not self.same_engine_sync:
                    continue
            if d.dma:
                dl.append(d)
            else:
                cur = latest.get(d.eng)
                if cur is None or d.gidx > cur.gidx:
                    latest[d.eng] = d
        dl.extend(latest.values())
        o.deps = dl
        self.ops.append(o)
        return o

    def coll(self, kind, groups, src, dst, reads=(), writes=()):
        o = self.op("gpsimd", lambda e: e.collective_compute(kind, ALU.bypass, replica_groups=groups, ins=[src],
                                                             outs=[dst]), reads, writes, dma=True)
        n = len([x for x in self.ops if x.dma and x.dsem is not None and x.dsem[0] == "coll"])
        o.dsem = ("coll", 0)
        o.dval = n + 1
        return o

    def dyn_dma(self, eng, fn, reads=(), writes=(), window=6):
        prior = [x for x in self.ops if x.dma and x.eng == eng and (x.dsem is None or x.dsem[0] != "coll")][-N_DMA_SEMS:]
        o = self.op(eng, fn, reads, writes, dma=True)
        o.deps.extend(prior)
        if not hasattr(self, "_dyn"):
            self._dyn = []
        if self._dyn:
            o.deps.append(self._dyn[-1])
        self._dyn.append(o)
        return o

    def dma(self, eng, out, in_, reads=(), writes=(), **kw):
        return self.op(eng, lambda e: e.dma_start(out=out, in_=in_, **kw), reads, writes, dma=True)

    def emit(self):
        nc = self.nc
        for o in self.ops:
            for d in o.deps:
                d.signal = True
        G = getattr(nc, "_gsem", None)
        if G is None:
            G = {"stack": ExitStack(), "sems": {}, "cnt": {e: 0 for e in self.ENGS}, "ep": {e: 0 for e in self.ENGS},
                 "dcount": {}, "coll": 0}
            setattr(nc, "_gsem", G)
        sems = G["sems"]

        def gsem(key, name):
            if key not in sems:
                sems[key] = G["stack"].enter_context(nc.semaphore(name))
            return sems[key]
        cnt, ep = G["cnt"], G["ep"]
        for o in self.ops:
            if o.dma or not o.signal:
                continue
            if cnt[o.eng] >= SEM_LIM:
                ep[o.eng] += 1
                cnt[o.eng] = 0
            cnt[o.eng] += 1
            o.epoch = ep[o.eng]
            o.idx = cnt[o.eng]
            gsem(("c", o.eng, o.epoch), f"c_{o.eng}_{o.epoch}")
        colls = [o for o in self.ops if o.dma and o.dsem is not None]
        for o in colls:
            G["coll"] += 1
            o.dval = G["coll"]
            gsem(("d", "coll", 0), "collsem")
        dma_prev = {}
        dcount = G["dcount"]
        for o in self.ops:
            if not o.dma or o.dsem is not None:
                continue
            i = dcount.get(o.eng, 0)
            dcount[o.eng] = i + 1
            slot = i % N_DMA_SEMS
            o.dsem = (o.eng, slot)
            o.dval = 16 * (i // N_DMA_SEMS + 1)
            prev = dma_prev.get(o.dsem)
            if prev is not None:
                o.deps.append(prev)
            dma_prev[o.dsem] = o
            gsem(("d", o.eng, slot), f"d_{o.eng}_{slot}")
        per_eng = {e: [] for e in self.ENGS}
        for o in self.ops:
            per_eng[o.eng].append(o)
        self.n_waits = 0

        def run_engine(ename, eh):
            waited = {}
            for o in per_eng[ename]:
                for d in o.deps:
                    if d.dma:
                        key = ("d",) + d.dsem
                        val = d.dval
                    else:
                        key = ("c", d.eng, d.epoch)
                        val = d.idx
                    if waited.get(key, 0) >= val:
                        continue
                    waited[key] = val
                    eh.wait_ge(sems[key], val)
                    self.n_waits += 1
                ins = o.fn(eh)
                if ins is None:
                    continue
                if o.dma and o.dsem[0] == "coll":
                    ins.then_inc(sems[("d",) + o.dsem])
                elif o.dma:
                    ins.then_inc(sems[("d",) + o.dsem], 16)
                elif o.signal:
                    ins.then_inc(sems[("c", o.eng, o.epoch)], 1)

        with nc.Block() as block:
            if per_eng["sync"]:
                @block.sync
                def _(e):
                    run_engine("sync", e)
            if per_eng["scalar"]:
                @block.scalar
                def _(e):
                    run_engine("scalar", e)
            if per_eng["vector"]:
                @block.vector
                def _(e):
                    run_engine("vector", e)
            if per_eng["gpsimd"]:
                @block.gpsimd
                def _(e):
                    run_engine("gpsimd", e)
            if per_eng["tensor"]:
                @block.tensor
                def _(e):
                    run_engine("tensor", e)
        self.stack.close()

    def psum_banks(self):
        return [(self.psum(f"ps{i}", [128, 512], F32), Buf(f"ps{i}", psum=True)) for i in range(8)]

    def finish_wait(self, eng, ops):
        o = self.op(eng, lambda e: None)
        o.deps = list(ops)


def copy_op(P, eng, out, in_, reads, writes):
    if eng == "scalar":
        return P.op(eng, lambda e: e.copy(out=out, in_=in_), reads, writes)
    return P.op(eng, lambda e: e.tensor_copy(out=out, in_=in_), reads, writes)


D = 1024
TOK = 4096
NT = TOK // 128
SEQ = 16384
EPS = 1e-6
NCOL = 2832


def new_nc():
    return bass.Bass("TRN2", target_bir_lowering=False)


def din(nc, name, shape, dt=F32):
    return nc.dram_tensor(name, list(shape), dt, kind="ExternalInput").ap()


def dout(nc, name, shape, dt=F32):
    return nc.dram_tensor(name, list(shape), dt, kind="ExternalOutput").ap()


class Ctx:
    def __init__(self, P):
        self.P = P
        self.rr = 0
        self.ld = 0

    def ev_eng(self):
        self.rr ^= 1
        return "vector" if self.rr else "scalar"

    def ld_eng(self):
        self.ld ^= 1
        return "sync" if self.ld else "gpsimd"


def T(P, name, shape, dt):
    return P.sbuf(name, shape, dt), Buf(name)


def emit_cond(P, dC, zeros=None):
    c_sb, c_b = T(P, "c_sb", [128, 8], F32)
    cond, cond_b = T(P, "cond", [128, 8], F32)
    P.dma("sync", c_sb[:], dC, writes=[c_b])
    P.op("scalar", lambda e: e.activation(out=cond[:], in_=c_sb[:], func=AF.Silu), reads=[c_b], writes=[cond_b])
    return cond, cond_b


def emit_ada_fm(P, cond, cond_b, dAW, dABT, j0, nj, ps, ps_b, name):
    res, res_b = T(P, name, [128, nj], F32)
    abt, abt_b = T(P, name + "_ab", [128, nj], F32)
    P.dma("sync", abt[:], dABT[:, j0:j0 + nj], writes=[abt_b])
    awv = dAW.rearrange("(k p) n -> p k n", p=128)
    stg = [T(P, f"{name}_aw{i}", [128, 8, 256], F32) for i in range(2)]
    for c0 in range(0, nj, 2):
        cn = min(2, nj - c0)
        st, st_b = stg[(c0 // 2) % 2]
        P.dma("sync" if (c0 // 2) % 2 == 0 else "gpsimd", st[:, :, :cn * 128],
              awv[:, :, (j0 + c0) * 128:(j0 + c0 + cn) * 128], writes=[st_b])
        for jj in range(cn):
            for kc in range(8):
                P.op("tensor", lambda e, jj=jj, kc=kc, st=st, c0=c0: e.matmul(
                    ps[:, c0 + jj:c0 + jj + 1], lhsT=st[:, kc, jj * 128:(jj + 1) * 128],
                    rhs=cond[:, kc:kc + 1], start=(kc == 0), stop=(kc == 7)),
                    reads=[st_b, cond_b], writes=[ps_b])
    P.op("vector", lambda e: e.tensor_tensor(out=res[:], in0=ps[:, :nj], in1=abt[:], op=ALU.add),
         reads=[ps_b, abt_b], writes=[res_b])
    return res, res_b


def emit_ada_bc(P, cond, cond_b, dAW, dAB_row, col0, ncols, pss, name, zeros, zeros_b):
    res, res_b = T(P, name, [128, ncols], F32)
    crep, crep_b = T(P, name + "_crep", [128, 8, 128], F32)
    for kc in range(8):
        P.op("scalar", lambda e, kc=kc: e.activation(out=crep[:, kc, :], in_=zeros[:, :128], func=AF.Identity,
                                                     bias=cond[:, kc:kc + 1], scale=1.0),
             reads=[cond_b, zeros_b], writes=[crep_b])
    abr, abr_b = T(P, name + "_abr", [128, ncols], F32)
    P.dma("sync", abr[:], dAB_row[:, col0:col0 + ncols].partition_broadcast(128), writes=[abr_b])
    awv = dAW.rearrange("(k p) n -> p k n", p=128)
    stg = [T(P, f"{name}_aw{i}", [128, 8, 512], F32) for i in range(2)]
    for ci, c0 in enumerate(range(0, ncols, 512)):
        st, st_b = stg[ci % 2]
        ps, ps_b = pss[ci % len(pss)]
        P.dma("sync" if ci % 2 == 0 else "gpsimd", st[:], awv[:, :, col0 + c0:col0 + c0 + 512], writes=[st_b])
        for kc in range(8):
            P.op("tensor", lambda e, kc=kc, st=st, ps=ps: e.matmul(
                ps[:, :512], lhsT=crep[:, kc, :], rhs=st[:, kc, :], start=(kc == 0), stop=(kc == 7)),
                reads=[st_b, crep_b], writes=[ps_b])
        P.op("vector", lambda e, ps=ps, c0=c0: e.tensor_tensor(out=res[:, c0:c0 + 512], in0=ps[:, :512],
                                                               in1=abr[:, c0:c0 + 512], op=ALU.add),
             reads=[ps_b, abr_b], writes=[res_b])
    return res, res_b


def emit_norm_hT(P, C, dXsrc, a_fm, a_b, sh_fm, sh_b, ident, ident_b, hT, hT_bufs, psT, tag,
                 n_tiles=NT, x_keep=None, hT32=None):
    xs = [T(P, f"{tag}_x{i}", [128, D], F32) for i in range(3)]
    xn = [T(P, f"{tag}_xn{i}", [128, D], F32) for i in range(2)]
    junk, junk_b = T(P, f"{tag}_junk", [128, D], BF16)
    st = [T(P, f"{tag}_st{i}", [128, 4], F32) for i in range(2)]
    for t in range(n_tiles):
        x_t, x_b = xs[t % 3]
        xn_t, xn_b = xn[t % 2]
        s_t, s_b = st[t % 2]
        P.dma(C.ld_eng(), x_t[:], dXsrc[t * 128:(t + 1) * 128, :], writes=[x_b])
        P.op("scalar", lambda e, x_t=x_t, s_t=s_t: e.activation(out=junk[:], in_=x_t[:], func=AF.Square,
                                                               accum_out=s_t[:, 0:1]),
             reads=[x_b], writes=[junk_b, s_b])
        P.op("scalar", lambda e, s_t=s_t: e.activation(out=s_t[:, 1:2], in_=s_t[:, 0:1], func=AF.Sqrt,
                                                       scale=1.0 / D, bias=EPS), reads=[s_b], writes=[s_b])
        P.op("vector", lambda e, s_t=s_t: e.reciprocal(out=s_t[:, 2:3], in_=s_t[:, 1:2]), reads=[s_b], writes=[s_b])
        P.op("vector", lambda e, x_t=x_t, xn_t=xn_t, s_t=s_t: e.tensor_scalar(
            out=xn_t[:], in0=x_t[:], scalar1=s_t[:, 2:3], scalar2=None, op0=ALU.mult),
            reads=[x_b, s_b], writes=[xn_b])
        SUB = int(os.environ.get("SUB", "9"))
        if SUB == 1:
            P.dbg_out.append(P.dma("sync", P.dbgx, xn_t[:], reads=[xn_b]))
            continue
        (pa, pa_b), (pb, pb_b) = psT[t % 2]
        for k in range(8):
            pp, pp_b = (pa, pa_b) if k < 4 else (pb, pb_b)
            P.op("tensor", lambda e, k=k, pp=pp, xn_t=xn_t: e.transpose(
                out=pp[:, (k % 4) * 128:(k % 4 + 1) * 128], in_=xn_t[:, k * 128:(k + 1) * 128], identity=ident[:]),
                reads=[xn_b, ident_b], writes=[pp_b])
        if SUB == 2:
            copy_op(P, "vector", x_t[:, 0:512], pa[:], [pa_b], [x_b])
            copy_op(P, "vector", x_t[:, 512:1024], pb[:], [pb_b], [x_b])
            P.dbg_out.append(P.dma("sync", P.dbgx, x_t[:], reads=[x_b]))
            continue
        for k in range(8):
            pp, pp_b = (pa, pa_b) if k < 4 else (pb, pb_b)
            src = pp[:, (k % 4) * 128:(k % 4 + 1) * 128]
            dst = hT[:, k, t * 128:(t + 1) * 128]
            if True:
                P.op("scalar", lambda e, k=k, src=src, dst=dst: e.activation(
                    out=dst, in_=src, func=AF.Identity, scale=a_fm[:, k:k + 1], bias=sh_fm[:, k:k + 1]),
                    reads=[pp_b, a_b, sh_b], writes=[hT_bufs[t][k]])
            else:
                P.op("vector", lambda e, k=k, src=src, dst=dst: e.tensor_scalar(
                    out=dst, in0=src, scalar1=a_fm[:, k:k + 1], scalar2=sh_fm[:, k:k + 1], op0=ALU.mult,
                    op1=ALU.add), reads=[pp_b, a_b, sh_b], writes=[hT_bufs[t][k]])
            if hT32 is not None:
                d32 = hT32[0][:, k, t * 128:(t + 1) * 128]
                P.op("vector", lambda e, k=k, src=src, d32=d32: e.tensor_scalar(
                    out=d32, in0=src, scalar1=a_fm[:, k:k + 1], scalar2=sh_fm[:, k:k + 1], op0=ALU.mult,
                    op1=ALU.add), reads=[pp_b, a_b, sh_b], writes=[hT32[1][t]])


def load_w_bf16(P, C, dW, kch, ncols, wb, wb_b, tag, col_chunk=256, cast_engs=("gpsimd", "vector")):
    wv = dW.rearrange("(k p) n -> p k n", p=128)
    stg = [T(P, f"{tag}_stg{i}", [128, kch, col_chunk], F32) for i in range(2)]
    for ci, c0 in enumerate(range(0, ncols, col_chunk)):
        cn = min(col_chunk, ncols - c0)
        st, st_b = stg[ci % 2]
        P.dma(C.ld_eng(), st[:, :, :cn], wv[:, :, c0:c0 + cn], writes=[st_b])
        ce = cast_engs[ci % len(cast_engs)]
        cb = Buf(f"{tag}_c{ci}")
        wb_b.append(cb)
        P.op(ce, lambda e, st=st, c0=c0, cn=cn: e.tensor_copy(out=wb[:, :, c0:c0 + cn], in_=st[:, :, :cn]),
             reads=[st_b], writes=[cb])


def build_A(stage=9):
    nc = new_nc()
    P = Prog(nc)
    C = Ctx(P)
    dX = din(nc, "x", [TOK, D])
    dC = din(nc, "cT", [128, 8])
    dAW = din(nc, "ada_w", [D, 6 * D])
    dABT = din(nc, "ada_bT", [128, 48])
    dNP = din(nc, "npreT", [128, 8])
    dWin = din(nc, "w_in", [D, NCOL])
    dId = din(nc, "ident", [128, 128])
    o_hT = dout(nc, "hT", [128, 8, TOK], BF16)
    o_qkT = dout(nc, "qkT", [4, 128, TOK], BF16)
    o_v = dout(nc, "vaug", [TOK, 4, 65], BF16)
    o_fm = dout(nc, "fmT", [12, 128, TOK], F32)
    o_dz = dout(nc, "dz", [TOK, 256], F32)
    o_dbg = dout(nc, "dbg", [TOK, 16], F32)
    P.dbgx = dout(nc, "dbgx", [128, 1024], F32)
    P.dbg_out = []

    ident, ident_b = T(P, "ident", [128, 128], F32)
    P.dma("sync", ident[:], dId, writes=[ident_b])
    pss = [(P.psum(f"ps{i}", [128, 512], F32), Buf(f"ps{i}")) for i in range(8)]

    cond, cond_b = emit_cond(P, dC)
    mod, mod_b = emit_ada_fm(P, cond, cond_b, dAW, dABT, 0, 16, pss[0][0], pss[0][1], "modA")
    npre, npre_b = T(P, "npre", [128, 8], F32)
    P.dma("sync", npre[:], dNP, writes=[npre_b])
    a1, a1_b = T(P, "a1", [128, 8], F32)
    P.op("vector", lambda e: e.scalar_tensor_tensor(out=a1[:], in0=mod[:, 8:16], scalar=1.0, in1=npre[:],
                                                    op0=ALU.add, op1=ALU.mult),
         reads=[mod_b, npre_b], writes=[a1_b])

    outs = []
    if stage == 1:
        outs.append(P.dma("sync", o_dbg[0:128, 0:8], a1[:], reads=[a1_b]))
        outs.append(P.dma("sync", o_dbg[0:128, 8:16], mod[:, 0:8], reads=[mod_b]))
        P.finish_wait("sync", outs)
        P.emit()
        return nc
    hT, _ = T(P, "hT", [128, 8, TOK], BF16)
    hT_bufs = [[Buf(f"hT{t}_{k}") for k in range(8)] for t in range(NT)]
    wb = P.sbuf("w_in_bf", [128, 8, NCOL], BF16)
    wb_b = []
    if stage != 22:
        load_w_bf16(P, C, dWin, 8, NCOL, wb, wb_b, "win")
    if stage == 21:
        outs.append(P.dma("sync", o_hT[:, :, 0:512], wb[:, :, 0:512], reads=wb_b))
        P.finish_wait("sync", outs)
        P.emit()
        return nc
    psT = [(pss[0], pss[1]), (pss[2], pss[3])]
    emit_norm_hT(P, C, dX, a1, a1_b, mod, mod_b, ident, ident_b, hT, hT_bufs, psT, "nA",
                 n_tiles=(int(os.environ.get("NTILES", "4")) if stage == 22 else NT))
    if stage == 22:
        if not P.dbg_out:
            outs.append(P.dma("sync", o_hT[:, :, 0:512], hT[:, :, 0:512], reads=[b for t in range(4) for b in hT_bufs[t]]))
        P.finish_wait("sync", outs + P.dbg_out)
        P.emit()
        return nc
    def store(dst, src, reads):
        outs.append(P.dma("gpsimd", dst, src, reads=reads))

    for c in range(8):
        store(o_hT[:, :, c * 512:(c + 1) * 512], hT[:, :, c * 512:(c + 1) * 512], [b for t in range(c * 4, c * 4 + 4) for b in hT_bufs[t]])

    if stage == 2:
        P.finish_wait("sync", outs)
        P.emit()
        return nc
    scale = 32 ** -0.5
    pj = [pss[4], pss[5], pss[6], pss[7]]
    pji = [0]

    def next_ps():
        r = pj[pji[0] % 4]
        pji[0] += 1
        return r

    ostg_f = [T(P, f"ostg_f{i}", [128, 512], F32) for i in range(4)]
    ostg_b = [T(P, f"ostg_b{i}", [128, 512], BF16) for i in range(3)]
    cnt = [0, 0]

    for tc in range(8):
        tsl = slice(tc * 512, (tc + 1) * 512)

        def fm(col0):
            ps, ps_b = next_ps()
            for kc in range(8):
                P.op("tensor", lambda e, kc=kc, ps=ps, col0=col0, tsl=tsl: e.matmul(
                    ps[:], lhsT=wb[:, kc, col0:col0 + 128], rhs=hT[:, kc, tsl], start=(kc == 0), stop=(kc == 7)),
                    reads=wb_b + [hT_bufs[tc * 4 + i][kc] for i in range(4)], writes=[ps_b])
            return ps, ps_b

        for g in range(4):
            ps, ps_b = fm(g * 128)
            st, st_b = ostg_b[cnt[0] % 3]
            cnt[0] += 1
            sc = scale if g < 2 else 1.0
            P.op("scalar", lambda e, ps=ps, st=st, sc=sc: e.activation(out=st[:], in_=ps[:], func=AF.Copy, scale=sc),
                 reads=[ps_b], writes=[st_b])
            store(o_qkT[g, :, tsl], st[:], [st_b])
        for j in range(4):
            ps, ps_b = fm(768 + j * 128)
            st, st_b = ostg_f[cnt[1] % 4]
            cnt[1] += 1
            copy_op(P, C.ev_eng(), st[:], ps[:], [ps_b], [st_b])
            store(o_fm[j if j < 2 else 8 + j, :, tsl], st[:], [st_b])
        for j in range(2):
            psc, psc_b = fm(1280 + j * 128)
            psx, psx_b = fm(1536 + j * 128)
            tmp, tmp_b = ostg_f[cnt[1] % 4]
            cnt[1] += 1
            st, st_b = ostg_f[cnt[1] % 4]
            cnt[1] += 1
            P.op("scalar", lambda e, psc=psc, tmp=tmp: e.copy(out=tmp[:], in_=psc[:]), reads=[psc_b], writes=[tmp_b])
            P.op("vector", lambda e, psx=psx, tmp=tmp, st=st: e.tensor_tensor(out=st[:], in0=psx[:], in1=tmp[:],
                                                                             op=ALU.mult),
                 reads=[psx_b, tmp_b], writes=[st_b])
            store(o_fm[2 + j, :, tsl], st[:], [st_b])
        for j in range(6):
            ps, ps_b = fm(1792 + j * 128)
            st, st_b = ostg_f[cnt[1] % 4]
            cnt[1] += 1
            copy_op(P, C.ev_eng(), st[:], ps[:], [ps_b], [st_b])
            store(o_fm[4 + j, :, tsl], st[:], [st_b])

    if stage == 3:
        P.finish_wait("sync", outs)
        P.emit()
        return nc
    vst = [T(P, f"vst{i}", [128, 4, 65], BF16) for i in range(2)]
    for i in range(2):
        P.op("gpsimd", lambda e, i=i: e.memset(vst[i][0][:], 1.0), writes=[vst[i][1]])
    zst = [T(P, f"zst{i}", [128, 272], F32) for i in range(2)]
    for t in range(NT):
        ps, ps_b = next_ps()
        for kc in range(8):
            P.op("tensor", lambda e, kc=kc, ps=ps, t=t: e.matmul(
                ps[:, 0:256], lhsT=hT[:, kc, t * 128:(t + 1) * 128], rhs=wb[:, kc, 512:768], start=(kc == 0),
                stop=(kc == 7)), reads=wb_b + [hT_bufs[t][kc]], writes=[ps_b])
        v_t, v_b = vst[t % 2]
        P.op("vector", lambda e, ps=ps, v_t=v_t: e.tensor_copy(
            out=v_t[:, :, 0:64], in_=ps[:, 0:256].rearrange("p (h d) -> p h d", h=4)), reads=[ps_b], writes=[v_b])
        store(o_v[t * 128:(t + 1) * 128], v_t[:], [v_b])
        ps, ps_b = next_ps()
        for kc in range(8):
            P.op("tensor", lambda e, kc=kc, ps=ps, t=t: e.matmul(
                ps[:, 0:272], lhsT=hT[:, kc, t * 128:(t + 1) * 128], rhs=wb[:, kc, 2560:2832], start=(kc == 0),
                stop=(kc == 7)), reads=wb_b + [hT_bufs[t][kc]], writes=[ps_b])
        z_t, z_b = zst[t % 2]
        P.op("scalar", lambda e, ps=ps, z_t=z_t: e.copy(out=z_t[:], in_=ps[:, 0:272]), reads=[ps_b], writes=[z_b])
        store(o_dz[t * 128:(t + 1) * 128, :], z_t[:, 0:256], [z_b])
        store(o_dbg[t * 128:(t + 1) * 128, :], z_t[:, 256:272], [z_b])
    P.finish_wait("sync", outs)
    P.emit()
    return nc


NH = 4
NK = 2 * 128 * TOK
NV = 4 * 128 * 32 * 65
SLOPES = [2.0 ** (-8.0 * (i + 1) / 4) for i in range(4)]


def att_host_tables(core):
    r = core % 4
    bf = ml_dtypes.bfloat16
    qf = np.arange(512)
    qaug = np.zeros((2, 8, 3, 512), np.float32)
    for qi in range(8):
        qaug[0, qi, 0] = -16.0 * (qf // 16)
        qaug[0, qi, 1] = -1.0 * (qf % 16)
        qaug[0, qi, 2] = -512.0 * qi
        qaug[1, qi] = -qaug[0, qi]
    kaug = np.zeros((4, 3, 4, TOK), np.float32)
    biasF = np.zeros((128, 4, 4, 32), np.float32)
    kp = np.arange(128)[:, None]
    kt = np.arange(32)[None, :]
    for h in range(4):
        m = SLOPES[h]
        for s in range(4):
            j = (r + s) % 4
            sgn = 1.0 if (s == 0 or j < r) else -1.0
            kaug[h, :, s, :] = sgn * m
            biasF[:, h, s, :] = sgn * m * (kt * 128 + kp + (j - r) * 4096)
    off = np.arange(4)[None, :, None]
    d = np.abs(qf[None, None, :] - np.arange(128)[:, None, None] - off * 128)
    dhi = -(16.0 * (d // 16))
    dlo = -1.0 * (d % 16)
    dtab = np.stack([dhi, dlo], 1).astype(np.float32)
    return {
        "qaug": qaug.astype(bf), "kaug": kaug.astype(bf), "biasF": biasF,
        "dtab": dtab.astype(bf), "identb": np.eye(128, dtype=np.float32).astype(bf),
    }


def emit_att(P, C, pss, d_qkT, d_kvall, d_oaT, d_lam, d_subln, tabs, ident, ident_b, lambda_init, outs,
             heads=range(4), qtiles=range(8), dyn_rank=True, kv_b=None):
    d_qaug, d_kaug, d_biasF, d_dtab, d_identb = tabs
    identb, identb_b = T(P, "identb", [128, 128], BF16)
    P.dma("sync", identb[:], d_identb, writes=[identb_b])
    dbase, dbase_b = T(P, "dbase", [128, 2, 4, 512], BF16)
    P.dma("sync", dbase[:], d_dtab, writes=[dbase_b])
    dtab_h, dtab_hb = T(P, "dtab_h", [128, 2, 4, 512], BF16)
    biasF, biasF_b = T(P, "biasF", [128, 4, 4, 32], F32)
    P.dma("sync", biasF[:], d_biasF, writes=[biasF_b])
    biasB, biasB_b = T(P, "biasB", [128, 4, 32], F32)
    P.op("vector", lambda e: e.tensor_scalar(out=biasB[:], in0=biasF[:, :, 0, :], scalar1=-1.0, scalar2=None,
                                             op0=ALU.mult), reads=[biasF_b], writes=[biasB_b])
    lamt, lamt_b = T(P, "lamt", [128, 4, 32], F32)
    P.dma("sync", lamt[:], d_lam.partition_broadcast(128), writes=[lamt_b])
    lw, lw_b = T(P, "lamw", [128, 8], F32)
    lj, lj_b = T(P, "lamj", [128, 2, 32], F32)
    P.op("vector", lambda e: e.tensor_tensor(out=lj[:, 0, :], in0=lamt[:, 0, :], in1=lamt[:, 1, :], op=ALU.mult),
         reads=[lamt_b], writes=[lj_b])
    P.op("vector", lambda e: e.tensor_tensor(out=lj[:, 1, :], in0=lamt[:, 2, :], in1=lamt[:, 3, :], op=ALU.mult),
         reads=[lamt_b, lj_b], writes=[lj_b])
    P.op("vector", lambda e: e.tensor_reduce(out=lw[:, 0:2], in_=lj[:], axis=AX.X, op=ALU.add), reads=[lj_b],
         writes=[lw_b])
    P.op("scalar", lambda e: e.activation(out=lw[:, 2:4], in_=lw[:, 0:2], func=AF.Exp), reads=[lw_b], writes=[lw_b])
    P.op("vector", lambda e: e.scalar_tensor_tensor(out=lw[:, 4:5], in0=lw[:, 3:4], scalar=-lambda_init, in1=lw[:, 2:3],
                                                    op0=ALU.add, op1=ALU.subtract), reads=[lw_b], writes=[lw_b])
    neg_lam = lw[:, 4:5]
    subln, subln_b = T(P, "subln", [128, 64], F32)
    P.dma("sync", subln[:], d_subln.partition_broadcast(128), writes=[subln_b])
    P.op("vector", lambda e: e.tensor_scalar(out=subln[:], in0=subln[:], scalar1=1.0 - lambda_init, scalar2=None,
                                             op0=ALU.mult), reads=[subln_b], writes=[subln_b])

    kvrot = P.dram("kvrot", [4, NK + NV], BF16).ap()
    rot_b = Buf("rot")
    nchk = d_kvall.shape[0]
    csz = (NK + NV) // nchk
    for c in range(nchk):
        for s in range(4):
            en = ("sync", "gpsimd")[(c * 4 + s) % 2]

            def cpkv(e, s=s, c=c, en=en):
                rk = P.rank_val(e, s, en) if dyn_rank else s
                return e.dma_start(out=kvrot[s, c * csz:(c + 1) * csz].rearrange("(q t) -> q t", q=128),
                                   in_=d_kvall[c, bass.ds(rk, 1), :].rearrange("a (q t) -> (a q) t", q=128))
            P.dyn_dma(en, cpkv, reads=[kv_b] if kv_b is not None else [], writes=[rot_b])

    def krot_v(s, h):
        return kvrot[s, 0:NK].rearrange("(m p t) -> m p t", m=2, p=128)[:, h * 32:(h + 1) * 32, :]

    def vrot_v(s, h):
        return kvrot[s, NK:NK + NV].rearrange("(h p t d) -> h p t d", h=4, p=128, t=32)[h]
    if os.environ.get('STOPROT'):
        return
    kT = P.sbuf("kT", [35, 2, 4, TOK], BF16)
    kT_b = Buf("kT")
    vS = P.sbuf("vS", [128, 4, 32, 65], BF16)
    vS_b = Buf("vS")
    qA = [T(P, f"qA{i}", [35, 2, 512], BF16) for i in range(2)]
    qB = [T(P, f"qB{i}", [35, 2, 512], BF16) for i in range(2)]
    pT = [T(P, f"pT{i}", [128, 512], BF16) for i in range(6)]
    oacc = P.sbuf("oacc", [128, 32, 256], F32)
    oacc_bufs = [Buf(f"oacc{i}") for i in range(32)]
    oTs = [T(P, f"oTs{i}", [65, 512], F32) for i in range(2)]
    ps_s = [pss[0], pss[1], pss[2], pss[3]]
    ps_o = [pss[4], pss[5]]
    ps_t = [pss[6], pss[7]]
    sm, sm_b = [], []
    for i in range(2):
        a, b = T(P, f"attsm{i}", [128, 8], F32)
        sm.append(a)
        sm_b.append(b)
    tmp1 = [T(P, f"atttmp{i}", [128, 64], F32) for i in range(2)]

    step = [0]
    for h in heads:
        m_h = SLOPES[h]
        P.op("gpsimd", lambda e, m_h=m_h: e.tensor_scalar(out=dtab_h[:], in0=dbase[:], scalar1=m_h, scalar2=None,
                                                         op0=ALU.mult), reads=[dbase_b], writes=[dtab_hb])
        for s in range(4):
            P.dma("sync", kT[0:32, :, s, :], krot_v(s, h).rearrange("m p t -> p m t"),
                  reads=[rot_b], writes=[kT_b])
            P.dma("sync", vS[:, s, :, :], vrot_v(s, h), reads=[rot_b], writes=[vS_b])
        for mp in range(2):
            P.dma("sync", kT[32:35, mp, :, :], d_kaug[h], writes=[kT_b])
        for qi in qtiles:
            qa, qa_b = qA[qi % 2]
            qb, qb_b = qB[qi % 2]
            qsl = slice(qi * 512, (qi + 1) * 512)
            src = d_qkT[0:2, h * 32:(h + 1) * 32, qsl].rearrange("m p t -> p m t")
            P.dma("gpsimd", qa[0:32, :, :], src, writes=[qa_b])
            P.dma("gpsimd", qb[0:32, :, :], src, writes=[qb_b])
            for mp in range(2):
                P.dma("gpsimd", qa[32:35, mp, :], d_qaug[0, qi], writes=[qa_b])
                P.dma("gpsimd", qb[32:35, mp, :], d_qaug[1, qi], writes=[qb_b])
            tiles = [(0, kt) for kt in range(32)] + [(s, kt) for s in range(1, 4) for kt in range(32)]
            steps = []
            for ti, (s, kt) in enumerate(tiles):
                if s == 0 and 4 * qi <= kt < 4 * qi + 4:
                    case = "C"
                elif s == 0 and kt >= 4 * qi + 4:
                    case = "B"
                else:
                    case = "A"
                for mp in range(2):
                    steps.append((s, kt, mp, case, ti == 0, ti == len(tiles) - 1))
            LAG = 3
            pend = []
            for idx in range(len(steps) + LAG):
                if idx < len(steps):
                    s, kt, mp, case, first, last = steps[idx]
                    pS, pS_b = ps_s[step[0] % 4]
                    pt, pt_b = pT[step[0] % 6]
                    step[0] += 1
                    ksl = slice(kt * 128, (kt + 1) * 128)
                    if case == "C":
                        off = kt - 4 * qi
                        P.op("tensor", lambda e, pS=pS, mp=mp, s=s, ksl=ksl, qa=qa: e.matmul(
                            pS[:], lhsT=kT[0:32, mp, s, ksl], rhs=qa[0:32, mp, :], start=True, stop=False),
                            reads=[kT_b, qa_b], writes=[pS_b])
                        P.op("tensor", lambda e, pS=pS, off=off: e.matmul(
                            pS[:], lhsT=identb[:], rhs=dtab_h[:, 0, off, :], start=False, stop=False),
                            reads=[identb_b, dtab_hb], writes=[pS_b])
                        P.op("tensor", lambda e, pS=pS, off=off: e.matmul(
                            pS[:], lhsT=identb[:], rhs=dtab_h[:, 1, off, :], start=False, stop=True),
                            reads=[identb_b, dtab_hb], writes=[pS_b])
                        P.op("scalar", lambda e, pS=pS, pt=pt: e.activation(out=pt[:], in_=pS[:], func=AF.Exp),
                             reads=[pS_b], writes=[pt_b])
                    else:
                        qq, qq_b = (qa, qa_b) if case == "A" else (qb, qb_b)
                        bcol = biasF[:, h, s, kt:kt + 1] if case == "A" else biasB[:, h, kt:kt + 1]
                        P.op("tensor", lambda e, pS=pS, mp=mp, s=s, ksl=ksl, qq=qq: e.matmul(
                            pS[:], lhsT=kT[0:35, mp, s, ksl], rhs=qq[0:35, mp, :], start=True, stop=True),
                            reads=[kT_b, qq_b], writes=[pS_b])
                        P.op("scalar", lambda e, pS=pS, pt=pt, bcol=bcol: e.activation(
                            out=pt[:], in_=pS[:], func=AF.Exp, bias=bcol, scale=1.0),
                            reads=[pS_b, biasF_b, biasB_b], writes=[pt_b])
                    pend.append((s, kt, mp, first, last, pt, pt_b))
                if idx >= LAG:
                    s, kt, mp, first, last, pt, pt_b = pend[idx - LAG]
                    po, po_b = ps_o[mp]
                    P.op("tensor", lambda e, po=po, pt=pt, s=s, kt=kt, first=first, last=last: e.matmul(
                        po[0:65, :], lhsT=vS[:, s, kt, :], rhs=pt[:], start=first, stop=last),
                        reads=[vS_b, pt_b], writes=[po_b])
            for mp in range(2):
                po, po_b = ps_o[mp]
                ot, ot_b = oTs[mp]
                copy_op(P, "vector" if mp == 0 else "scalar", ot[:], po[0:65, :], [po_b], [ot_b])
            for sub in range(4):
                g = qi * 4 + sub
                pt0, pt0_b = ps_t[0]
                pt1, pt1_b = ps_t[1]
                for mp, (ptt, ptt_b) in enumerate(((pt0, pt0_b), (pt1, pt1_b))):
                    ot, ot_b = oTs[mp]
                    P.op("tensor", lambda e, ptt=ptt, ot=ot, sub=sub: e.transpose(
                        out=ptt[:, 0:65], in_=ot[:, sub * 128:(sub + 1) * 128], identity=ident[0:65, 0:65]),
                        reads=[ot_b, ident_b], writes=[ptt_b])
                smt, smtb = sm[g % 2], sm_b[g % 2]
                t1, t1_b = tmp1[g % 2]
                P.op("vector", lambda e, smt=smt, pt0=pt0: e.reciprocal(out=smt[:, 0:1], in_=pt0[:, 64:65]),
                     reads=[pt0_b], writes=[smtb])
                P.op("vector", lambda e, smt=smt, pt1=pt1: e.reciprocal(out=smt[:, 1:2], in_=pt1[:, 64:65]),
                     reads=[pt1_b, smtb], writes=[smtb])
                P.op("vector", lambda e, smt=smt: e.tensor_tensor(out=smt[:, 2:3], in0=smt[:, 1:2], in1=neg_lam,
                                                                  op=ALU.mult), reads=[smtb, lw_b], writes=[smtb])
                P.op("vector", lambda e, smt=smt, pt0=pt0, t1=t1: e.tensor_scalar(
                    out=t1[:], in0=pt0[:, 0:64], scalar1=smt[:, 0:1], scalar2=None, op0=ALU.mult),
                    reads=[pt0_b, smtb], writes=[t1_b])
                P.op("vector", lambda e, smt=smt, pt1=pt1, t1=t1, g=g, h=h: e.scalar_tensor_tensor(
                    out=oacc[:, g, h * 64:(h + 1) * 64], in0=pt1[:, 0:64], scalar=smt[:, 2:3], in1=t1[:],
                    op0=ALU.mult, op1=ALU.add), reads=[pt1_b, smtb, t1_b], writes=[oacc_bufs[g]])

    junk, junk_b = T(P, "attjunk", [128, 64], F32)
    ss = [T(P, f"attss{i}", [128, 12], F32) for i in range(2)]
    on = [T(P, f"atton{i}", [128, 256], F32) for i in range(2)]
    ob = [T(P, f"attob{i}", [128, 2, 128], BF16) for i in range(2)]
    for g in range(32):
        if g // 4 not in qtiles:
            continue
        s_t, s_b = ss[g % 2]
        on_t, on_b = on[g % 2]
        ob_t, ob_b = ob[g % 2]
        for h in range(4):
            P.op("scalar", lambda e, g=g, h=h, s_t=s_t: e.activation(
                out=junk[:], in_=oacc[:, g, h * 64:(h + 1) * 64], func=AF.Square, accum_out=s_t[:, h:h + 1]),
                reads=[oacc_bufs[g]], writes=[junk_b, s_b])
        P.op("scalar", lambda e, s_t=s_t: e.activation(out=s_t[:, 4:8], in_=s_t[:, 0:4], func=AF.Sqrt, scale=1.0 / 64,
                                                       bias=EPS), reads=[s_b], writes=[s_b])
        P.op("vector", lambda e, s_t=s_t: e.reciprocal(out=s_t[:, 8:12], in_=s_t[:, 4:8]), reads=[s_b], writes=[s_b])
        for h in range(4):
            P.op("vector", lambda e, g=g, h=h, s_t=s_t, on_t=on_t: e.scalar_tensor_tensor(
                out=on_t[:, h * 64:(h + 1) * 64], in0=oacc[:, g, h * 64:(h + 1) * 64], scalar=s_t[:, 8 + h:9 + h],
                in1=subln[:], op0=ALU.mult, op1=ALU.mult), reads=[oacc_bufs[g], s_b, subln_b], writes=[on_b])
        ptt, ptt_b = ps_t[g % 2]
        for c2 in range(2):
            P.op("tensor", lambda e, ptt=ptt, on_t=on_t, c2=c2: e.transpose(
                out=ptt[:, c2 * 128:(c2 + 1) * 128], in_=on_t[:, c2 * 128:(c2 + 1) * 128], identity=ident[:]),
                reads=[on_b, ident_b], writes=[ptt_b])
        copy_op(P, "scalar", ob_t[:], ptt[:, 0:256].rearrange("p (c t) -> p c t", c=2), [ptt_b], [ob_b])
        outs.append(P.dma("gpsimd", d_oaT[:, :, g * 128:(g + 1) * 128].rearrange("c p t -> p c t"), ob_t[:],
                          reads=[ob_b]))


def build_ATT(lambda_init, heads=range(4), qtiles=range(8)):
    nc = new_nc()
    P = Prog(nc)
    C = Ctx(P)
    d_qkT = din(nc, "qkT", [4, 128, TOK], BF16)
    d_kvall = din(nc, "kvall", [4, NK + NV], BF16)
    d_lam = din(nc, "dlam", [4, 32])
    d_subln = din(nc, "subln", [64])
    d_qaug = din(nc, "qaug", [2, 8, 3, 512], BF16)
    d_kaug = din(nc, "kaug", [4, 3, 4, TOK], BF16)
    d_biasF = din(nc, "biasF", [128, 4, 4, 32])
    d_dtab = din(nc, "dtab", [128, 2, 4, 512], BF16)
    d_identb = din(nc, "identb", [128, 128], BF16)
    d_id = din(nc, "ident", [128, 128])
    d_oaT = dout(nc, "oaT", [2, 128, TOK], BF16)
    ident, ident_b = T(P, "ident", [128, 128], F32)
    P.dma("sync", ident[:], d_id, writes=[ident_b])
    pss = [(P.psum(f"ps{i}", [128, 512], F32), Buf(f"ps{i}")) for i in range(8)]
    outs = []
    emit_att(P, C, pss, d_qkT, d_kvall, d_oaT, d_lam, d_subln, (d_qaug, d_kaug, d_biasF, d_dtab, d_identb),
             ident, ident_b, lambda_init, outs, heads=heads, qtiles=qtiles)
    P.finish_wait("sync", outs)
    P.emit()
    return nc


NCH = SEQ // 64
NG = NCH // 8
DW = 196


def delta_host_consts():
    i = np.arange(64)
    c = np.zeros((64, 9, 64), np.float32)
    c[:, 0] = np.eye(64)
    c[:, 1] = 1.0
    c[:, 2] = -1.0
    c[:, 3] = 1.0 - np.eye(64)
    for d in range(2):
        allowed = (i[None, :] <= i[:, None]) if d == 0 else (i[None, :] >= i[:, None])
        c[:, 4 + d] = allowed.T.astype(np.float32)
        c[:, 6 + d] = np.where(allowed, 0.0, -1.0e4)
    return {"dconst": c}


def emit_delta(P, C, pss, d_loc, d_o, d_const, ident, ident_b, outs, ngroups=NG, loc_b=None):
    G = 8
    cst, cst_b = T(P, "dcst", [64, 9, 64], F32)
    P.dma("sync", cst[:], d_const, writes=[cst_b])
    I64 = cst[:, 0, :]
    ONES = cst[:, 1, :]
    NEG1 = cst[:, 2, :]
    rep, rep_b = T(P, "drep", [64, 5, G, 64], F32)
    for ri, ci in enumerate((0, 3, 6, 7, 1)):
        P.op("vector", lambda e, ri=ri, ci=ci: e.tensor_copy(
            out=rep[:, ri, :, :], in_=cst[:, ci, :].unsqueeze(1).to_broadcast([64, G, 64])),
            reads=[cst_b], writes=[rep_b])
    Irep = rep[:, 0]
    Offrep = rep[:, 1]
    Onesrep = rep[:, 4]

    def bc_c(ap2):
        return ap2.unsqueeze(2).to_broadcast([64, G, 64])

    Sst = [[T(P, f"dS{ch}_{p}", [64, 64], F32) for p in range(2)] for ch in range(2)]
    for ch in range(2):
        P.op("gpsimd", lambda e, ch=ch: e.memset(Sst[ch][0][0][:], 0.0), writes=[Sst[ch][0][1]])
    scount = [0, 0]

    NB = 2
    names = ["X", "dec", "decT", "kT", "qT", "qgT", "nkbT", "nkb", "qg", "Xv", "Xw", "kd", "gB", "gU",
             "P", "PT", "Q", "QT", "P2", "PT2", "R", "Rp", "R2", "Rp2", "attnT", "u", "wT", "osb"]
    tl = {}
    dbl = ("u", "wT", "qgT", "attnT", "kd", "osb")
    for ch in range(2):
        for n in names:
            shp = [64, G, DW] if n == "X" else [64, G, 64]
            for b in range(NB):
                if b == 0 or n in dbl:
                    tl[(n, ch, b)] = T(P, f"d{n}{ch}{b}", shp, F32)
                else:
                    tl[(n, ch, b)] = tl[(n, ch, 0)]
        for b in range(NB):
            tl[("sc", ch, b)] = T(P, f"dsc{ch}{b}", [64, 64], F32)
    vnew = [[T(P, f"dvn{ch}_{p}", [64, 64], F32) for p in range(2)] for ch in range(2)]
    prep_ps = [pss[i] for i in range(4)]
    ppi = [0]

    def nps():
        r = prep_ps[ppi[0] % 4]
        ppi[0] += 1
        return r
    small_b = [[Buf(f"ps1_{ch}"), Buf(f"ps2_{ch}")] for ch in range(2)]
    ps_o = [pss[6], pss[7]]
    evi = [0]

    def ev():
        evi[0] += 1
        return ("vector", "scalar")[evi[0] % 2]

    STG = int(os.environ.get('DSTAGE', '9'))

    def prep(ch, gi, b):
        d = ch
        tok0 = gi * 512
        X, X_b = tl[("X", ch, b)]
        P.dma("sync" if ch == 0 else "gpsimd", X[:], d_loc[tok0:tok0 + 512, :].rearrange("(c i) f -> i c f", i=64),
              reads=[loc_b] if loc_b is not None else [], writes=[X_b])
        q = X[:, :, 0:64]
        k = X[:, :, 64:128]
        v = X[:, :, 128:192]
        beta = X[:, :, 192 + d]
        g = X[:, :, 194 + d]
        sc, sc_b = tl[("sc", ch, b)]
        Cm = cst[:, 4 + d, :]
        NegMrep = rep[:, 2 + d]
        pa, pa_b = nps()
        P.op("tensor", lambda e: e.matmul(pa[0:64, 0:8], lhsT=Cm, rhs=g, start=True, stop=True),
             reads=[cst_b, X_b], writes=[pa_b])
        P.op("tensor", lambda e: e.matmul(pa[0:64, 8:16], lhsT=ONES, rhs=g, start=True, stop=True),
             reads=[cst_b, X_b], writes=[pa_b])
        P.op("vector", lambda e: e.tensor_copy(out=sc[:, 0:16], in_=pa[0:64, 0:16]), reads=[pa_b], writes=[sc_b])
        P.op("vector", lambda e: e.tensor_tensor(out=sc[:, 16:24], in0=sc[:, 8:16], in1=sc[:, 0:8], op=ALU.subtract),
             reads=[sc_b], writes=[sc_b])
        P.op("scalar", lambda e: e.activation(out=sc[:, 24:48], in_=sc[:, 0:24], func=AF.Exp), reads=[sc_b],
             writes=[sc_b])
        egc = sc[:, 24:32]
        egl = sc[:, 32:40]
        edl = sc[:, 40:48]
        P.op("vector", lambda e: e.tensor_tensor(out=sc[:, 48:56], in0=beta, in1=egc, op=ALU.mult),
             reads=[sc_b, X_b], writes=[sc_b])
        P.op("vector", lambda e: e.tensor_scalar(out=sc[:, 56:64], in0=beta, scalar1=-1.0, scalar2=None, op0=ALU.mult),
             reads=[sc_b, X_b], writes=[sc_b])
        bexp = sc[:, 48:56]
        nbeta = sc[:, 56:64]
        if STG <= 1:
            return None
        gB, gB_b = tl[("gB", ch, b)]
        gU, gU_b = tl[("gU", ch, b)]
        P.op("vector", lambda e: e.tensor_tensor(out=gB[:], in0=Onesrep, in1=bc_c(g), op=ALU.mult),
             reads=[rep_b, X_b], writes=[gB_b])
        P.op("gpsimd", lambda e: e.tensor_tensor(out=gU[:], in0=gB[:], in1=Cm.unsqueeze(1).to_broadcast([64, G, 64]),
                                                 op=ALU.mult), reads=[gB_b, cst_b], writes=[gU_b])
        pd, pd_b = nps()
        fl = "p c j -> p (c j)"
        P.op("tensor", lambda e: e.matmul(pd[0:64, :], lhsT=Cm, rhs=gB[:].rearrange(fl), start=True, stop=False),
             reads=[cst_b, gB_b], writes=[pd_b])
        P.op("tensor", lambda e: e.matmul(pd[0:64, :], lhsT=NEG1, rhs=gU[:].rearrange(fl), start=False, stop=False),
             reads=[cst_b, gU_b], writes=[pd_b])
        P.op("tensor", lambda e: e.matmul(pd[0:64, :], lhsT=I64, rhs=NegMrep.rearrange(fl), start=False, stop=True),
             reads=[cst_b, rep_b], writes=[pd_b])
        dec, dec_b = tl[("dec", ch, b)]
        P.op("scalar", lambda e: e.activation(out=dec[:].rearrange(fl), in_=pd[0:64, :], func=AF.Exp),
             reads=[pd_b], writes=[dec_b])
        if STG <= 2:
            return None
        nkb, nkb_b = tl[("nkb", ch, b)]
        qg, qg_b = tl[("qg", ch, b)]
        Xv, Xv_b = tl[("Xv", ch, b)]
        Xw, Xw_b = tl[("Xw", ch, b)]
        kd, kd_b = tl[("kd", ch, b)]
        P.op("vector", lambda e: e.tensor_tensor(out=nkb[:], in0=k, in1=bc_c(nbeta), op=ALU.mult),
             reads=[X_b, sc_b], writes=[nkb_b])
        P.op("gpsimd", lambda e: e.tensor_tensor(out=qg[:], in0=q, in1=bc_c(egc), op=ALU.mult),
             reads=[X_b, sc_b], writes=[qg_b])
        P.op("vector", lambda e: e.tensor_tensor(out=Xv[:], in0=v, in1=bc_c(beta), op=ALU.mult),
             reads=[X_b], writes=[Xv_b])
        P.op("gpsimd", lambda e: e.tensor_tensor(out=Xw[:], in0=k, in1=bc_c(bexp), op=ALU.mult),
             reads=[X_b, sc_b], writes=[Xw_b])
        P.op("vector", lambda e: e.tensor_tensor(out=kd[:], in0=k, in1=bc_c(edl), op=ALU.mult),
             reads=[X_b, sc_b], writes=[kd_b])
        if STG <= 3:
            return None
        tr = {}
        for nm, src, src_b in (("kT", k, X_b), ("qT", q, X_b), ("qgT", qg[:], qg_b), ("nkbT", nkb[:], nkb_b),
                               ("decT", dec[:], dec_b)):
            pp, pp_b = nps()
            for c in range(G):
                P.op("tensor", lambda e, pp=pp, src=src, c=c: e.transpose(
                    out=pp[0:64, c * 64:(c + 1) * 64], in_=src[:, c, :], identity=ident[0:64, 0:64]),
                    reads=[src_b, ident_b], writes=[pp_b])
            dst, dst_b = tl[(nm, ch, b)]
            copy_op(P, "scalar" if nm in ("kT", "qgT") else ev(), dst[:].rearrange(fl), pp[0:64, :], [pp_b], [dst_b])
            tr[nm] = (dst, dst_b)
        kT, kT_b = tr["kT"]
        qT, qT_b = tr["qT"]
        qgT, qgT_b = tr["qgT"]
        nkbT, nkbT_b = tr["nkbT"]
        decT, decT_b = tr["decT"]
        if STG <= 4:
            return None
        Pc, Pc_b = tl[("P", ch, b)]
        PTc, PTc_b = tl[("PT", ch, b)]
        attnT, attnT_b = tl[("attnT", ch, b)]
        pA, pA_b = nps()
        pAT, pAT_b = nps()
        pAt, pAt_b = nps()
        for c in range(G):
            cs = slice(c * 64, (c + 1) * 64)
            P.op("tensor", lambda e, c=c, cs=cs: e.matmul(pA[0:64, cs], lhsT=nkbT[:, c, :], rhs=kT[:, c, :],
                                                          start=True, stop=True),
                 reads=[nkbT_b, kT_b], writes=[pA_b])
            P.op("tensor", lambda e, c=c, cs=cs: e.matmul(pAT[0:64, cs], lhsT=kT[:, c, :], rhs=nkbT[:, c, :],
                                                          start=True, stop=True),
                 reads=[nkbT_b, kT_b], writes=[pAT_b])
            P.op("tensor", lambda e, c=c, cs=cs: e.matmul(pAt[0:64, cs], lhsT=kT[:, c, :], rhs=qT[:, c, :],
                                                          start=True, stop=True),
                 reads=[qT_b, kT_b], writes=[pAt_b])
        P.op("vector", lambda e: e.tensor_tensor(out=Pc[:].rearrange(fl), in0=pA[0:64, :], in1=dec[:].rearrange(fl),
                                                 op=ALU.mult), reads=[pA_b, dec_b], writes=[Pc_b])
        P.op("gpsimd", lambda e: e.tensor_tensor(out=Pc[:], in0=Pc[:], in1=Offrep, op=ALU.mult),
             reads=[Pc_b, rep_b], writes=[Pc_b])
        P.op("vector", lambda e: e.tensor_tensor(out=PTc[:].rearrange(fl), in0=pAT[0:64, :], in1=decT[:].rearrange(fl),
                                                 op=ALU.mult), reads=[pAT_b, decT_b], writes=[PTc_b])
        P.op("gpsimd", lambda e: e.tensor_tensor(out=PTc[:], in0=PTc[:], in1=Offrep, op=ALU.mult),
             reads=[PTc_b, rep_b], writes=[PTc_b])
        P.op("vector", lambda e: e.tensor_tensor(out=attnT[:].rearrange(fl), in0=pAt[0:64, :],
                                                 in1=decT[:].rearrange(fl), op=ALU.mult),
             reads=[pAt_b, decT_b], writes=[attnT_b])
        if STG <= 5:
            return None
        R, R_b = tl[("R", ch, b)]
        Rp, Rp_b = tl[("Rp", ch, b)]
        P.op("gpsimd", lambda e: e.tensor_tensor(out=R[:], in0=PTc[:], in1=Irep, op=ALU.add),
             reads=[PTc_b, rep_b], writes=[R_b])
        P.op("vector", lambda e: e.tensor_tensor(out=Rp[:], in0=Pc[:], in1=Irep, op=ALU.add),
             reads=[Pc_b, rep_b], writes=[Rp_b])
        curP, curPT = (Pc, Pc_b), (PTc, PTc_b)
        nxtP, nxtPT = tl[("P2", ch, b)], tl[("PT2", ch, b)]
        curR, curRp = (R, R_b), (Rp, Rp_b)
        nxtR, nxtRp = tl[("R2", ch, b)], tl[("Rp2", ch, b)]
        Q, Q_b = tl[("Q", ch, b)]
        QT, QT_b = tl[("QT", ch, b)]
        DLVL = float(os.environ.get('DLVL', '9'))
        for lvl in range(1, 6):
            if lvl > DLVL:
                break
            p1, p1_b = nps()
            p2, p2_b = nps()
            (cP, cP_b), (cPT, cPT_b) = curP, curPT
            for c in range(G):
                cs = slice(c * 64, (c + 1) * 64)
                P.op("tensor", lambda e, c=c, cs=cs, cP=cP, cPT=cPT, p1=p1: e.matmul(
                    p1[0:64, cs], lhsT=cPT[:, c, :], rhs=cP[:, c, :], start=True, stop=True),
                    reads=[cP_b, cPT_b], writes=[p1_b])
                P.op("tensor", lambda e, c=c, cs=cs, cP=cP, cPT=cPT, p2=p2: e.matmul(
                    p2[0:64, cs], lhsT=cP[:, c, :], rhs=cPT[:, c, :], start=True, stop=True),
                    reads=[cP_b, cPT_b], writes=[p2_b])
            (nP, nP_b), (nPT, nPT_b) = nxtP, nxtPT
            if lvl < 5:
                copy_op(P, "scalar", nP[:].rearrange(fl), p1[0:64, :], [p1_b], [nP_b])
                copy_op(P, "vector", nPT[:].rearrange(fl), p2[0:64, :], [p2_b], [nPT_b])
                P.op("gpsimd", lambda e, nP=nP: e.tensor_tensor(out=Q[:], in0=nP[:], in1=Irep, op=ALU.add),
                     reads=[nP_b, rep_b], writes=[Q_b])
                P.op("gpsimd", lambda e, nPT=nPT: e.tensor_tensor(out=QT[:], in0=nPT[:], in1=Irep, op=ALU.add),
                     reads=[nPT_b, rep_b], writes=[QT_b])
            else:
                P.op("vector", lambda e, p1=p1: e.tensor_tensor(out=Q[:].rearrange(fl), in0=p1[0:64, :],
                                                                in1=Irep.rearrange(fl), op=ALU.add),
                     reads=[p1_b, rep_b], writes=[Q_b])
                P.op("vector", lambda e, p2=p2: e.tensor_tensor(out=QT[:].rearrange(fl), in0=p2[0:64, :],
                                                                in1=Irep.rearrange(fl), op=ALU.add),
                     reads=[p2_b, rep_b], writes=[QT_b])
            curP, curPT, nxtP, nxtPT = nxtP, nxtPT, curP, curPT
            p3, p3_b = nps()
            (cR, cR_b), (cRp, cRp_b) = curR, curRp
            (nR, nR_b), (nRp, nRp_b) = nxtR, nxtRp
            for c in range(G):
                cs = slice(c * 64, (c + 1) * 64)
                P.op("tensor", lambda e, c=c, cs=cs, cRp=cRp, p3=p3: e.matmul(
                    p3[0:64, cs], lhsT=cRp[:, c, :], rhs=QT[:, c, :], start=True, stop=True),
                    reads=[cRp_b, QT_b], writes=[p3_b])
            copy_op(P, "scalar", nR[:].rearrange(fl), p3[0:64, :], [p3_b], [nR_b])
            if lvl < 5:
                p4, p4_b = nps()
                for c in range(G):
                    cs = slice(c * 64, (c + 1) * 64)
                    P.op("tensor", lambda e, c=c, cs=cs, cR=cR, p4=p4: e.matmul(
                        p4[0:64, cs], lhsT=cR[:, c, :], rhs=Q[:, c, :], start=True, stop=True),
                        reads=[cR_b, Q_b], writes=[p4_b])
                copy_op(P, ev(), nRp[:].rearrange(fl), p4[0:64, :], [p4_b], [nRp_b])
            curR, curRp, nxtR, nxtRp = nxtR, nxtRp, curR, curRp
        (Rf, Rf_b) = curR
        if os.environ.get('DBGR') and ch == 0 and gi == 0:
            dbgsel = os.environ.get('DBGR')
            srcT, srcB = {"R": (Rf, Rf_b), "P0": tl[("P", ch, b)] if False else (Pc, Pc_b), "dec": (dec, dec_b), "attnT": (attnT, attnT_b), "Q": (Q, Q_b), "QT": (QT, QT_b), "P2": tl[("P2", ch, b)], "PT2": tl[("PT2", ch, b)], "Rp": curRp}[dbgsel]
            outs.append(P.dma("sync", d_o[1, 0:512, :].rearrange("(c i) f -> i c f", i=64), srcT[:], reads=[srcB]))
        if STG <= 6:
            return None
        u, u_b = tl[("u", ch, b)]
        wT, wT_b = tl[("wT", ch, b)]
        pu, pu_b = nps()
        pw, pw_b = nps()
        for c in range(G):
            cs = slice(c * 64, (c + 1) * 64)
            P.op("tensor", lambda e, c=c, cs=cs, Rf=Rf: e.matmul(pu[0:64, cs], lhsT=Rf[:, c, :], rhs=Xv[:, c, :],
                                                               start=True, stop=True),
                 reads=[Rf_b, Xv_b], writes=[pu_b])
            P.op("tensor", lambda e, c=c, cs=cs, Rf=Rf: e.matmul(pw[0:64, cs], lhsT=Xw[:, c, :], rhs=Rf[:, c, :],
                                                               start=True, stop=True),
                 reads=[Rf_b, Xw_b], writes=[pw_b])
        copy_op(P, "scalar", u[:].rearrange(fl), pu[0:64, :], [pu_b], [u_b])
        copy_op(P, ev(), wT[:].rearrange(fl), pw[0:64, :], [pw_b], [wT_b])
        return dict(u=(u, u_b), wT=(wT, wT_b), qgT=(qgT, qgT_b), attnT=(attnT, attnT_b), kd=(kd, kd_b),
                    egl=(egl, sc_b), tok0=tok0)

    def rec(ch, pr, c, b, first_in_group):
        u, u_b = pr["u"]
        wT, wT_b = pr["wT"]
        qgT, qgT_b = pr["qgT"]
        attnT, attnT_b = pr["attnT"]
        kd, kd_b = pr["kd"]
        egl, egl_b = pr["egl"]
        n = scount[ch]
        scount[ch] += 1
        S, S_b = Sst[ch][n % 2]
        S2, S2_b = Sst[ch][(n + 1) % 2]
        vn, vn_b = vnew[ch][n % 2]
        b1, b2 = small_b[ch]
        ps1 = pss[4 + ch][0][0:64, 0:64]
        ps2 = pss[4 + ch][0][0:64, 64:128]
        po, po_b = ps_o[ch]
        P.op("tensor", lambda e: e.matmul(ps1, lhsT=wT[:, c, :], rhs=S[:], start=True, stop=True),
             reads=[wT_b, S_b], writes=[b1])
        P.op("vector", lambda e: e.tensor_tensor(out=vn[:], in0=u[:, c, :], in1=ps1, op=ALU.subtract),
             reads=[u_b, b1], writes=[vn_b])
        P.op("tensor", lambda e: e.matmul(ps2, lhsT=kd[:, c, :], rhs=vn[:], start=True, stop=True),
             reads=[kd_b, vn_b], writes=[b2])
        cs = slice(c * 64, (c + 1) * 64)
        P.op("tensor", lambda e: e.matmul(po[0:64, cs], lhsT=qgT[:, c, :], rhs=S[:], start=True, stop=False),
             reads=[qgT_b, S_b], writes=[po_b])
        P.op("tensor", lambda e: e.matmul(po[0:64, cs], lhsT=attnT[:, c, :], rhs=vn[:], start=False, stop=True),
             reads=[attnT_b, vn_b], writes=[po_b])
        P.op("vector", lambda e: e.scalar_tensor_tensor(out=S2[:], in0=S[:], scalar=egl[:, c:c + 1], in1=ps2,
                                                        op0=ALU.mult, op1=ALU.add),
             reads=[S_b, egl_b, b2], writes=[S2_b])

    for gi in range(ngroups):
        b = gi % NB
        prs = []
        for ch in range(2):
            g_idx = gi if ch == 0 else NG - 1 - gi
            prs.append(prep(ch, g_idx, b))
        if STG <= 7:
            continue
        for cc in range(G):
            for ch in range(2):
                c = cc if ch == 0 else G - 1 - cc
                rec(ch, prs[ch], c, b, cc == 0)
        for ch in range(2):
            po, po_b = ps_o[ch]
            osb, osb_b = tl[("osb", ch, b)]
            copy_op(P, "scalar", osb[:].rearrange("p c j -> p (c j)"), po[0:64, :], [po_b], [osb_b])
            tok0 = prs[ch]["tok0"]
            dst = d_o(ch, tok0) if callable(d_o) else d_o[ch, tok0:tok0 + 512, :]
            outs.append(P.dma("sync" if ch == 0 else "gpsimd", dst.rearrange("(c i) f -> i c f", i=64), osb[:],
                              reads=[osb_b]))


def build_DELTA(ngroups=NG):
    nc = new_nc()
    P = Prog(nc)
    C = Ctx(P)
    d_loc = din(nc, "dloc", [SEQ, DW])
    d_const = din(nc, "dconst", [64, 9, 64])
    d_id = din(nc, "ident", [128, 128])
    d_o = dout(nc, "do", [2, SEQ, 64])
    ident, ident_b = T(P, "ident", [128, 128], F32)
    P.dma("sync", ident[:], d_id, writes=[ident_b])
    pss = [(P.psum(f"ps{i}", [128, 512], F32), Buf(f"ps{i}")) for i in range(8)]
    outs = []
    emit_delta(P, C, pss, d_loc, d_o, d_const, ident, ident_b, outs, ngroups=ngroups)
    P.finish_wait("sync", outs)
    P.emit()
    return nc


HAL = 8
POOL_W = (2, 4, 8, 16)


def b_host_tables(core, inp, l):
    r = core % 4
    masks = np.zeros((128, 2), np.float32)
    masks[:, 0] = 0.0 if r == 0 else 1.0
    masks[:, 1] = 0.0 if r == 3 else 1.0
    corr = np.ones((128, 2, 16), np.float32)
    for ch in range(2):
        for half in range(2):
            w = POOL_W[ch * 2 + half]
            ps = slice(half * 64, (half + 1) * 64)
            for i in range(8):
                if r == 0:
                    t = i
                    cnt = min(t + w // 2, SEQ) - max(t - w // 2, 0)
                    corr[ps, ch, i] = w / cnt
                if r == 3:
                    t = SEQ - 8 + i
                    cnt = min(t + w // 2, SEQ) - max(t - w // 2, 0)
                    corr[ps, ch, 8 + i] = w / cnt
    invw = np.zeros((128, 2), np.float32)
    for ch in range(2):
        for half in range(2):
            invw[half * 64:(half + 1) * 64, ch] = 1.0 / POOL_W[ch * 2 + half]
    pw = np.zeros((2, 128, 128), np.float32)
    for g in range(4):
        ch, half = g // 2, g % 2
        pw[ch, half * 64:(half + 1) * 64, half * 64:(half + 1) * 64] = inp["pool_w"][l, g]
    return {
        "bmask": masks, "bcorr": corr, "binvw": invw, "poolw": pw,
        "pscale": np.ascontiguousarray(inp["pool_scale"][l].reshape(2, 128).T),
        "sconvT": np.ascontiguousarray(inp["sconv_w"][l].reshape(3, 2, 128).transpose(2, 1, 0)),
        "dconvT": np.ascontiguousarray(inp["delta_conv_w"][l].reshape(5, 6, 128).transpose(2, 1, 0)),
        "alog": np.ascontiguousarray(inp["delta_a_log"][l].reshape(8)),
        "dtb": np.ascontiguousarray(inp["delta_dt_bias"][l].reshape(8)),
    }


def emit_edges(P, d_fmT, d_edge, fm_b):
    eb = Buf("edge")
    P.dma("sync", d_edge[:, :, 0:HAL], d_fmT[0:10, :, 0:HAL], reads=[fm_b] if fm_b else [], writes=[eb])
    o = P.dma("gpsimd", d_edge[:, :, HAL:2 * HAL], d_fmT[0:10, :, TOK - HAL:TOK], reads=[fm_b] if fm_b else [],
              writes=[eb])
    return eb


def emit_b(P, C, pss, d_fmT, d_edge_all, d_dbg, d_obT, d_ocT, d_send, tabs, ident, ident_b, outs, dyn_rank=True,
           in_b=None):
    (d_mask, d_corr, d_invw, d_poolw, d_pscale, d_sconv, d_dconv, d_alog, d_dtb) = tabs
    rd = [in_b] if in_b is not None else []
    W = TOK + 2 * HAL
    hal, hal_b = T(P, "hal", [128, 2, 10, HAL], F32)

    def ldl(e):
        rk = P.rank_val(e, 3, "sync") if dyn_rank else 0
        return e.dma_start(out=hal[:, 0, :, :], in_=d_edge_all[bass.ds(rk, 1), :, :, HAL:2 * HAL].rearrange(
            "a j p c -> p (a j) c"))

    def ldr(e):
        rk = P.rank_val(e, 1, "gpsimd") if dyn_rank else 0
        return e.dma_start(out=hal[:, 1, :, :], in_=d_edge_all[bass.ds(rk, 1), :, :, 0:HAL].rearrange(
            "a j p c -> p (a j) c"))
    P.dyn_dma("sync", ldl, reads=rd, writes=[hal_b])
    P.dyn_dma("gpsimd", ldr, reads=rd, writes=[hal_b])
    small = {}
    for nm, dd, shp in (("mask", d_mask, [128, 2]), ("corr", d_corr, [128, 2, 16]), ("invw", d_invw, [128, 2]),
                        ("pscale", d_pscale, [128, 2]), ("sconv", d_sconv, [128, 2, 3]), ("dconv", d_dconv, [128, 6, 5])):
        t_, b_ = T(P, "b_" + nm, shp, F32)
        P.dma("sync", t_[:], dd, writes=[b_])
        small[nm] = (t_, b_)
    mask, mask_b = small["mask"]
    for side in range(2):
        P.op("vector", lambda e, side=side: e.tensor_scalar(out=hal[:, side], in0=hal[:, side],
                                                            scalar1=mask[:, side:side + 1], scalar2=None, op0=ALU.mult),
             reads=[hal_b, mask_b], writes=[hal_b])
    poolw, poolw_b = T(P, "b_poolw", [128, 2, 128], F32)
    P.dma("sync", poolw[:], d_poolw.rearrange("c p e -> p c e"), writes=[poolw_b])

    zt = [T(P, f"b_z{i}", [128, W], F32) for i in range(2)]
    wa, wa_b = T(P, "b_wa", [128, W], F32)
    wb, wb_b = T(P, "b_wb", [128, W], F32)
    zi = [0]

    def load_chunk(j):
        z, z_b = zt[zi[0] % 2]
        zi[0] += 1
        P.dma("sync", z[:, HAL:HAL + TOK], d_fmT[j], reads=rd, writes=[z_b])
        P.op("gpsimd", lambda e: e.tensor_copy(out=z[:, 0:HAL], in_=hal[:, 0, j, :]), reads=[hal_b], writes=[z_b])
        P.op("gpsimd", lambda e: e.tensor_copy(out=z[:, HAL + TOK:W], in_=hal[:, 1, j, :]), reads=[hal_b], writes=[z_b])
        return z, z_b

    ostg = [T(P, f"b_o{i}", [128, 512], BF16) for i in range(3)]
    oi = [0]
    corr, corr_b = small["corr"]
    invw, invw_b = small["invw"]
    pscale, pscale_b = small["pscale"]
    for ch in range(2):
        z, z_b = load_chunk(ch)
        P.op("vector", lambda e, z=z: e.tensor_tensor(out=wa[:, 1:W], in0=z[:, 0:W - 1], in1=z[:, 1:W], op=ALU.add),
             reads=[z_b], writes=[wa_b])
        P.op("vector", lambda e: e.tensor_tensor(out=wb[:, 2:W - 1], in0=wa[:, 1:W - 2], in1=wa[:, 3:W], op=ALU.add),
             reads=[wa_b], writes=[wb_b])
        if ch == 0:
            P.op("gpsimd", lambda e: e.tensor_copy(out=wb[0:64, HAL:HAL + TOK], in_=wa[0:64, HAL:HAL + TOK]),
                 reads=[wa_b, wb_b], writes=[wb_b])
            res, res_b = wb, wb_b
        else:
            P.op("vector", lambda e: e.tensor_tensor(out=wa[:, 4:W - 3], in0=wb[:, 2:W - 5], in1=wb[:, 6:W - 1],
                                                     op=ALU.add), reads=[wb_b, wa_b], writes=[wa_b])
            P.op("vector", lambda e: e.tensor_tensor(out=wb[64:128, 8:W - 7], in0=wa[64:128, 4:W - 11],
                                                     in1=wa[64:128, 12:W - 3], op=ALU.add),
                 reads=[wa_b, wb_b], writes=[wb_b])
            P.op("gpsimd", lambda e: e.tensor_copy(out=wb[0:64, HAL:HAL + TOK], in_=wa[0:64, HAL:HAL + TOK]),
                 reads=[wa_b, wb_b], writes=[wb_b])
            res, res_b = wb, wb_b
        P.op("vector", lambda e, ch=ch: e.tensor_scalar(out=res[:, HAL:HAL + TOK], in0=res[:, HAL:HAL + TOK],
                                                        scalar1=invw[:, ch:ch + 1], scalar2=None, op0=ALU.mult),
             reads=[res_b, invw_b], writes=[res_b])
        P.op("vector", lambda e, ch=ch: e.tensor_tensor(out=res[:, HAL:HAL + 8], in0=res[:, HAL:HAL + 8],
                                                        in1=corr[:, ch, 0:8], op=ALU.mult),
             reads=[res_b, corr_b], writes=[res_b])
        P.op("vector", lambda e, ch=ch: e.tensor_tensor(out=res[:, HAL + TOK - 8:HAL + TOK],
                                                        in0=res[:, HAL + TOK - 8:HAL + TOK], in1=corr[:, ch, 8:16],
                                                        op=ALU.mult), reads=[res_b, corr_b], writes=[res_b])
        P.op("vector", lambda e, z=z: e.tensor_tensor(out=res[:, HAL:HAL + TOK], in0=res[:, HAL:HAL + TOK],
                                                      in1=z[:, HAL:HAL + TOK], op=ALU.subtract),
             reads=[res_b, z_b], writes=[res_b])
        for tc in range(8):
            ps, ps_b = pss[tc % 2]
            P.op("tensor", lambda e, ps=ps, ch=ch, tc=tc: e.matmul(
                ps[:], lhsT=poolw[:, ch, :], rhs=res[:, HAL + tc * 512:HAL + (tc + 1) * 512], start=True, stop=True),
                reads=[poolw_b, res_b], writes=[ps_b])
            st, st_b = ostg[oi[0] % 3]
            oi[0] += 1
            P.op("scalar", lambda e, ps=ps, st=st, ch=ch: e.activation(out=st[:], in_=ps[:], func=AF.Copy,
                                                                      scale=pscale[:, ch:ch + 1]),
                 reads=[ps_b, pscale_b], writes=[st_b])
            outs.append(P.dma("gpsimd", d_obT[ch, :, tc * 512:(tc + 1) * 512], st[:], reads=[st_b]))
    sconv, sconv_b = small["sconv"]
    for ch in range(2):
        z, z_b = load_chunk(2 + ch)
        P.op("vector", lambda e, z=z, ch=ch: e.tensor_scalar(out=wa[:, HAL:HAL + TOK], in0=z[:, HAL - 1:HAL - 1 + TOK],
                                                             scalar1=sconv[:, ch, 0:1], scalar2=None, op0=ALU.mult),
             reads=[z_b, sconv_b], writes=[wa_b])
        for j in (1, 2):
            P.op("vector", lambda e, z=z, ch=ch, j=j: e.scalar_tensor_tensor(
                out=wa[:, HAL:HAL + TOK], in0=z[:, HAL - 1 + j:HAL - 1 + j + TOK], scalar=sconv[:, ch, j:j + 1],
                in1=wa[:, HAL:HAL + TOK], op0=ALU.mult, op1=ALU.add), reads=[z_b, sconv_b, wa_b], writes=[wa_b])
        P.dma("sync", wb[:, 0:TOK], d_fmT[10 + ch], reads=rd, writes=[wb_b])
        for tc in range(8):
            st, st_b = ostg[oi[0] % 3]
            oi[0] += 1
            P.op("gpsimd", lambda e, st=st, tc=tc: e.tensor_tensor(
                out=st[:], in0=wa[:, HAL + tc * 512:HAL + (tc + 1) * 512], in1=wb[:, tc * 512:(tc + 1) * 512],
                op=ALU.mult), reads=[wa_b, wb_b], writes=[st_b])
            outs.append(P.dma("gpsimd", d_ocT[ch, :, tc * 512:(tc + 1) * 512], st[:], reads=[st_b]))
    dconv, dconv_b = small["dconv"]
    dcv = P.dram("dcv", [6, 128, TOK], F32).ap()
    dcv_b = Buf("dcv")
    for j6 in range(6):
        z, z_b = load_chunk(4 + j6)
        P.op("vector", lambda e, z=z, j6=j6: e.tensor_scalar(out=wa[:, 0:TOK], in0=z[:, HAL - 2:HAL - 2 + TOK],
                                                             scalar1=dconv[:, j6, 0:1], scalar2=None, op0=ALU.mult),
             reads=[z_b, dconv_b], writes=[wa_b])
        for j in range(1, 5):
            eng = "vector"
            P.op(eng, lambda e, z=z, j6=j6, j=j: e.scalar_tensor_tensor(
                out=wa[:, 0:TOK], in0=z[:, HAL - 2 + j:HAL - 2 + j + TOK], scalar=dconv[:, j6, j:j + 1],
                in1=wa[:, 0:TOK], op0=ALU.mult, op1=ALU.add), reads=[z_b, dconv_b, wa_b], writes=[wa_b])
        P.op("scalar", lambda e: e.activation(out=wb[:, 0:TOK], in_=wa[:, 0:TOK], func=AF.Silu), reads=[wa_b],
             writes=[wb_b])
        P.dma("sync", dcv[j6], wb[:, 0:TOK], reads=[wb_b], writes=[dcv_b])
    arow, arow_b = T(P, "b_arow", [128, 16], F32)
    P.dma("sync", arow[:, 0:8], d_alog.partition_broadcast(128), writes=[arow_b])
    P.dma("sync", arow[:, 8:16], d_dtb.partition_broadcast(128), writes=[arow_b])
    P.op("scalar", lambda e: e.activation(out=arow[:, 0:8], in_=arow[:, 0:8], func=AF.Exp), reads=[arow_b],
         writes=[arow_b])
    P.op("vector", lambda e: e.tensor_scalar(out=arow[:, 0:8], in0=arow[:, 0:8], scalar1=-1.0, scalar2=None,
                                             op0=ALU.mult), reads=[arow_b], writes=[arow_b])
    fmt = [T(P, f"b_fm{i}", [128, 6, 128], F32) for i in range(2)]
    tm = [T(P, f"b_tm{i}", [128, 768], F32) for i in range(2)]
    sq, sq_b = T(P, "b_sq", [128, 512], F32)
    st8 = [T(P, f"b_s8{i}", [128, 48], F32) for i in range(2)]
    snd = [T(P, f"b_snd{i}", [128, 4, DW], F32) for i in range(2)]
    gbt = [T(P, f"b_gb{i}", [128, 16], F32) for i in range(2)]
    for t in range(NT):
        f_t, f_b = fmt[t % 2]
        m_t, m_b = tm[t % 2]
        s_t, s_b = st8[t % 2]
        o_t, o_b = snd[t % 2]
        g_t, g_b = gbt[t % 2]
        P.dma("sync", f_t[:], dcv[:, :, t * 128:(t + 1) * 128].rearrange("j p t -> p j t"), reads=[dcv_b], writes=[f_b])
        P.dma("gpsimd", g_t[:], d_dbg[t * 128:(t + 1) * 128, :], reads=rd, writes=[g_b])
        pa, pa_b = pss[2 + (t % 2) * 2]
        pb, pb_b = pss[3 + (t % 2) * 2]
        for j6 in range(6):
            pp, pp_b = (pa, pa_b) if j6 < 4 else (pb, pb_b)
            P.op("tensor", lambda e, pp=pp, f_t=f_t, j6=j6: e.transpose(
                out=pp[:, (j6 % 4) * 128:(j6 % 4 + 1) * 128], in_=f_t[:, j6, :], identity=ident[:]),
                reads=[f_b, ident_b], writes=[pp_b])
        copy_op(P, "scalar", m_t[:, 0:512], pa[:], [pa_b], [m_b])
        copy_op(P, "vector", m_t[:, 512:768], pb[:, 0:256], [pb_b], [m_b])
        P.op("gpsimd", lambda e, m_t=m_t: e.tensor_tensor(out=sq[:], in0=m_t[:, 0:512], in1=m_t[:, 0:512], op=ALU.mult),
             reads=[m_b], writes=[sq_b])
        P.op("vector", lambda e, s_t=s_t: e.tensor_reduce(out=s_t[:, 0:8], in_=sq[:].rearrange("p (g d) -> p g d", d=64),
                                                          axis=AX.X, op=ALU.add), reads=[sq_b], writes=[s_b])
        P.op("scalar", lambda e, s_t=s_t: e.activation(out=s_t[:, 8:16], in_=s_t[:, 0:8], func=AF.Sqrt, scale=1.0,
                                                       bias=EPS), reads=[s_b], writes=[s_b])
        P.op("vector", lambda e, s_t=s_t: e.reciprocal(out=s_t[:, 16:24], in_=s_t[:, 8:16]), reads=[s_b], writes=[s_b])
        P.op("vector", lambda e, s_t=s_t: e.tensor_scalar(out=s_t[:, 16:20], in0=s_t[:, 16:20], scalar1=64 ** -0.5,
                                                          scalar2=None, op0=ALU.mult), reads=[s_b], writes=[s_b])
        P.op("vector", lambda e, m_t=m_t, s_t=s_t, o_t=o_t: e.tensor_tensor(
            out=o_t[:, :, 0:64], in0=m_t[:, 0:256].rearrange("p (h d) -> p h d", d=64),
            in1=s_t[:, 16:20].unsqueeze(2).to_broadcast([128, 4, 64]), op=ALU.mult), reads=[m_b, s_b], writes=[o_b])
        P.op("gpsimd", lambda e, m_t=m_t, s_t=s_t, o_t=o_t: e.tensor_tensor(
            out=o_t[:, :, 64:128], in0=m_t[:, 256:512].rearrange("p (h d) -> p h d", d=64),
            in1=s_t[:, 20:24].unsqueeze(2).to_broadcast([128, 4, 64]), op=ALU.mult), reads=[m_b, s_b, o_b], writes=[o_b])
        P.op("gpsimd", lambda e, m_t=m_t, o_t=o_t: e.tensor_copy(
            out=o_t[:, :, 128:192], in_=m_t[:, 512:768].rearrange("p (h d) -> p h d", d=64)), reads=[m_b, o_b],
            writes=[o_b])
        P.op("scalar", lambda e, g_t=g_t, s_t=s_t: e.activation(out=s_t[:, 24:32], in_=g_t[:, 0:8], func=AF.Sigmoid),
             reads=[g_b], writes=[s_b])
        P.op("vector", lambda e, g_t=g_t, s_t=s_t: e.tensor_tensor(out=s_t[:, 32:40], in0=g_t[:, 8:16],
                                                                  in1=arow[:, 8:16], op=ALU.add),
             reads=[g_b, arow_b, s_b], writes=[s_b])
        P.op("scalar", lambda e, s_t=s_t: e.activation(out=s_t[:, 32:40], in_=s_t[:, 32:40], func=AF.Exp),
             reads=[s_b], writes=[s_b])
        P.op("scalar", lambda e, s_t=s_t: e.activation(out=s_t[:, 32:40], in_=s_t[:, 32:40], func=AF.Ln, bias=1.0,
                                                       scale=1.0), reads=[s_b], writes=[s_b])
        P.op("vector", lambda e, s_t=s_t: e.tensor_tensor(out=s_t[:, 40:48], in0=s_t[:, 32:40], in1=arow[:, 0:8],
                                                          op=ALU.mult), reads=[s_b, arow_b], writes=[s_b])
        P.op("vector", lambda e, s_t=s_t, o_t=o_t: e.tensor_copy(
            out=o_t[:, :, 192:194], in_=s_t[:, 24:32].rearrange("p (d h) -> p h d", d=2)), reads=[s_b, o_b],
            writes=[o_b])
        P.op("vector", lambda e, s_t=s_t, o_t=o_t: e.tensor_copy(
            out=o_t[:, :, 194:196], in_=s_t[:, 40:48].rearrange("p (d h) -> p h d", d=2)), reads=[s_b, o_b],
            writes=[o_b])
        outs.append(P.dma("gpsimd", d_send[t // 2, :, (t % 2) * 128:(t % 2 + 1) * 128, :].rearrange("h t f -> t h f"),
                          o_t[:], reads=[o_b]))


def build_B():
    nc = new_nc()
    P = Prog(nc)
    C = Ctx(P)
    d_fmT = din(nc, "fmT", [12, 128, TOK])
    d_edge_all = din(nc, "edge_all", [4, 10, 128, 16])
    d_dbg = din(nc, "dbg", [TOK, 16])
    tabs = (din(nc, "bmask", [128, 2]), din(nc, "bcorr", [128, 2, 16]), din(nc, "binvw", [128, 2]),
            din(nc, "poolw", [2, 128, 128]), din(nc, "pscale", [128, 2]), din(nc, "sconvT", [128, 2, 3]),
            din(nc, "dconvT", [128, 6, 5]), din(nc, "alog", [8]), din(nc, "dtb", [8]))
    d_id = din(nc, "ident", [128, 128])
    d_obT = dout(nc, "obT", [2, 128, TOK], BF16)
    d_ocT = dout(nc, "ocT", [2, 128, TOK], BF16)
    d_send = dout(nc, "dsend", [16, 4, 256, DW])
    ident, ident_b = T(P, "ident", [128, 128], F32)
    P.dma("sync", ident[:], d_id, writes=[ident_b])
    pss = [(P.psum(f"ps{i}", [128, 512], F32), Buf(f"ps{i}")) for i in range(8)]
    outs = []
    emit_b(P, C, pss, d_fmT, d_edge_all, d_dbg, d_obT, d_ocT, d_send, tabs, ident, ident_b, outs)
    P.finish_wait("sync", outs)
    P.emit()
    return nc


DFF = 2816
NFC = DFF // 128


def emit_rowbc(P, d_vec, n, name, eng="sync"):
    t_, b_ = T(P, name, [128, n], F32)
    P.dma(eng, t_[:], d_vec.partition_broadcast(128), writes=[b_])
    return t_, b_


def emit_c1(P, C, pss, d_x, d_hT, d_oaT, d_obT, d_ocT, d_dloc_o, d_dz, d_xmid, d_cT, d_aw, d_abrow, d_npost, d_dnorm,
            d_wmerge, d_bmergeT, d_wbranch, d_wo, ident, ident_b, outs, in_b=None):
    rd = [in_b] if in_b is not None else []
    zeros, zeros_b = T(P, "c1_zeros", [128, 128], F32)
    P.op("gpsimd", lambda e: e.memset(zeros[:], 0.0), writes=[zeros_b])
    cond, cond_b = emit_cond(P, d_cT)
    g1, g1_b = emit_ada_bc(P, cond, cond_b, d_aw, d_abrow, 2 * D, D, [pss[0], pss[1]], "c1g1", zeros, zeros_b)
    npost, npost_b = emit_rowbc(P, d_npost, D, "c1npost")
    P.op("vector", lambda e: e.tensor_tensor(out=g1[:], in0=g1[:], in1=npost[:], op=ALU.mult), reads=[g1_b, npost_b],
         writes=[g1_b])
    dnorm, dnorm_b = emit_rowbc(P, d_dnorm, 64, "c1dnorm")
    wm = P.sbuf("c1_wm", [128, 4, 8, D], BF16)
    wm_b = []
    for i in range(4):
        load_w_bf16(P, C, d_wmerge[i], 8, D, wm[:, i], wm_b, f"c1wm{i}")
    wbr = P.sbuf("c1_wbr", [128, 4, 2, D], BF16)
    wbr_b = []
    for i in range(4):
        load_w_bf16(P, C, d_wbranch[i], 2, D, wbr[:, i], wbr_b, f"c1wb{i}")
    wo = P.sbuf("c1_wo", [128, 8, D], BF16)
    wo_b = []
    load_w_bf16(P, C, d_wo, 8, D, wo, wo_b, "c1wo")
    bm, bm_b = T(P, "c1_bm", [128, 4, 8], F32)
    P.dma("sync", bm[:], d_bmergeT, writes=[bm_b])

    hTc = [T(P, f"c1_hT{i}", [128, 8, 512], BF16) for i in range(2)]
    brT = [T(P, f"c1_br{i}", [128, 4, 2, 512], BF16) for i in range(2)]
    mT = [T(P, f"c1_mT{i}", [128, 8, 512], BF16) for i in range(2)]
    gate = [T(P, f"c1_gate{i}", [128, 512], F32) for i in range(2)]
    acc = [T(P, f"c1_acc{i}", [128, 512], F32) for i in range(2)]
    odt = [T(P, f"c1_od{i}", [128, 4, 2, 64], F32) for i in range(2)]
    dzt = [T(P, f"c1_dz{i}", [128, 256], F32) for i in range(2)]
    ods = [T(P, f"c1_ods{i}", [128, 256], F32) for i in range(2)]
    st = [T(P, f"c1_st{i}", [128, 16], F32) for i in range(2)]
    xt = [T(P, f"c1_x{i}", [128, D], F32) for i in range(2)]
    ft = [T(P, f"c1_f{i}", [128, D], F32) for i in range(2)]
    junk, junk_b = T(P, "c1_junk", [128, D], BF16)
    gi = [0]
    for tc in range(8):
        tsl = slice(tc * 512, (tc + 1) * 512)
        h_t, h_b = hTc[tc % 2]
        b_t, b_b = brT[tc % 2]
        m_t, m_b = mT[tc % 2]
        P.dma("sync", h_t[:], d_hT[:, :, tsl], reads=rd, writes=[h_b])
        for i, dsrc in enumerate((d_oaT, d_obT, d_ocT)):
            P.dma("gpsimd", b_t[:, i], dsrc[:, :, tsl].rearrange("c p t -> p c t"), reads=rd, writes=[b_b])
        for sub in range(4):
            t = tc * 4 + sub
            o_t, o_b = odt[t % 2]
            z_t, z_b = dzt[t % 2]
            s_t, s_b = st[t % 2]
            d_t, d_b = ods[t % 2]
            P.dma("sync", o_t[:], d_dloc_o[:, :, t * 128:(t + 1) * 128, :].rearrange("h d t f -> t h d f"), reads=rd,
                  writes=[o_b])
            P.dma("gpsimd", z_t[:], d_dz[t * 128:(t + 1) * 128, :], reads=rd, writes=[z_b])
            P.op("vector", lambda e, o_t=o_t, d_t=d_t: e.tensor_tensor(
                out=d_t[:].rearrange("p (h f) -> p h f", h=4), in0=o_t[:, :, 0, :], in1=o_t[:, :, 1, :], op=ALU.add),
                reads=[o_b], writes=[d_b])
            P.op("gpsimd", lambda e, o_t=o_t, d_t=d_t: e.tensor_tensor(
                out=o_t[:].rearrange("p h d f -> p (h d f)")[:, 0:256], in0=d_t[:], in1=d_t[:], op=ALU.mult),
                reads=[d_b, o_b], writes=[o_b])
            P.op("vector", lambda e, o_t=o_t, s_t=s_t: e.tensor_reduce(
                out=s_t[:, 0:4], in_=o_t[:].rearrange("p h d f -> p (h d f)")[:, 0:256].rearrange("p (h f) -> p h f", h=4),
                axis=AX.X, op=ALU.add), reads=[o_b], writes=[s_b])
            P.op("scalar", lambda e, s_t=s_t: e.activation(out=s_t[:, 4:8], in_=s_t[:, 0:4], func=AF.Sqrt, scale=1.0 / 64,
                                                           bias=EPS), reads=[s_b], writes=[s_b])
            P.op("vector", lambda e, s_t=s_t: e.reciprocal(out=s_t[:, 8:12], in_=s_t[:, 4:8]), reads=[s_b], writes=[s_b])
            P.op("scalar", lambda e, z_t=z_t: e.activation(out=z_t[:], in_=z_t[:], func=AF.Silu), reads=[z_b],
                 writes=[z_b])
            P.op("vector", lambda e, d_t=d_t, s_t=s_t: e.tensor_tensor(
                out=d_t[:].rearrange("p (h f) -> p h f", h=4), in0=d_t[:].rearrange("p (h f) -> p h f", h=4),
                in1=s_t[:, 8:12].unsqueeze(2).to_broadcast([128, 4, 64]), op=ALU.mult), reads=[d_b, s_b], writes=[d_b])
            P.op("gpsimd", lambda e, d_t=d_t: e.tensor_tensor(
                out=d_t[:].rearrange("p (h f) -> p h f", h=4), in0=d_t[:].rearrange("p (h f) -> p h f", h=4),
                in1=dnorm[:].unsqueeze(1).to_broadcast([128, 4, 64]), op=ALU.mult), reads=[d_b, dnorm_b], writes=[d_b])
            P.op("vector", lambda e, d_t=d_t, z_t=z_t: e.tensor_tensor(out=d_t[:], in0=d_t[:], in1=z_t[:], op=ALU.mult),
                 reads=[d_b, z_b], writes=[d_b])
            pp, pp_b = pss[2 + (t % 2)]
            for c2 in range(2):
                P.op("tensor", lambda e, pp=pp, d_t=d_t, c2=c2: e.transpose(
                    out=pp[:, c2 * 128:(c2 + 1) * 128], in_=d_t[:, c2 * 128:(c2 + 1) * 128], identity=ident[:]),
                    reads=[d_b, ident_b], writes=[pp_b])
            copy_op(P, "scalar", b_t[:, 3, :, sub * 128:(sub + 1) * 128],
                    pp[:, 0:256].rearrange("p (c t) -> p c t", c=2), [pp_b], [b_b])
        for oc in range(8):
            osl = slice(oc * 128, (oc + 1) * 128)
            a_t, a_b = acc[gi[0] % 2]
            for i in range(4):
                pg, pg_b = pss[4 + (gi[0] % 2)]
                pb_, pb_b = pss[6 + (gi[0] % 2)]
                g_t, g_b = gate[gi[0] % 2]
                gi[0] += 1
                for kc in range(8):
                    P.op("tensor", lambda e, pg=pg, i=i, kc=kc, osl=osl, h_t=h_t: e.matmul(
                        pg[:], lhsT=wm[:, i, kc, osl], rhs=h_t[:, kc, :], start=(kc == 0), stop=(kc == 7)),
                        reads=wm_b + [h_b], writes=[pg_b])
                for c2 in range(2):
                    P.op("tensor", lambda e, pb_=pb_, i=i, c2=c2, osl=osl, b_t=b_t: e.matmul(
                        pb_[:], lhsT=wbr[:, i, c2, osl], rhs=b_t[:, i, c2, :], start=(c2 == 0), stop=(c2 == 1)),
                        reads=wbr_b + [b_b], writes=[pb_b])
                P.op("scalar", lambda e, pg=pg, g_t=g_t, i=i, oc=oc: e.activation(
                    out=g_t[:], in_=pg[:], func=AF.Sigmoid, bias=bm[:, i, oc:oc + 1], scale=1.0),
                    reads=[pg_b, bm_b], writes=[g_b])
                if i == 0:
                    P.op("vector", lambda e, pb_=pb_, g_t=g_t, a_t=a_t: e.tensor_tensor(
                        out=a_t[:], in0=pb_[:], in1=g_t[:], op=ALU.mult), reads=[pb_b, g_b], writes=[a_b])
                else:
                    P.op("vector", lambda e, pb_=pb_, g_t=g_t: e.tensor_tensor(
                        out=g_t[:], in0=pb_[:], in1=g_t[:], op=ALU.mult), reads=[pb_b, g_b], writes=[g_b])
                    if i < 3:
                        P.op("gpsimd", lambda e, g_t=g_t, a_t=a_t: e.tensor_tensor(
                            out=a_t[:], in0=a_t[:], in1=g_t[:], op=ALU.add), reads=[a_b, g_b], writes=[a_b])
                    else:
                        P.op("gpsimd", lambda e, g_t=g_t, a_t=a_t, m_t=m_t, oc=oc: e.tensor_tensor(
                            out=m_t[:, oc, :], in0=a_t[:], in1=g_t[:], op=ALU.add), reads=[a_b, g_b], writes=[m_b])
        for sub in range(4):
            t = tc * 4 + sub
            x_t, x_b = xt[t % 2]
            f_t, f_b = ft[t % 2]
            s_t, s_b = st[t % 2]
            P.dma("sync", x_t[:], d_x[t * 128:(t + 1) * 128, :], reads=rd, writes=[x_b])
            for half in range(2):
                pf, pf_b = pss[half]
                for kc in range(8):
                    P.op("tensor", lambda e, pf=pf, kc=kc, half=half, sub=sub, m_t=m_t: e.matmul(
                        pf[:], lhsT=m_t[:, kc, sub * 128:(sub + 1) * 128], rhs=wo[:, kc, half * 512:(half + 1) * 512],
                        start=(kc == 0), stop=(kc == 7)), reads=wo_b + [m_b], writes=[pf_b])
                P.op("scalar", lambda e, pf=pf, half=half, f_t=f_t: e.copy(out=f_t[:, half * 512:(half + 1) * 512],
                                                                          in_=pf[:]), reads=[pf_b], writes=[f_b])
            emit_post(P, f_t, f_b, x_t, x_b, g1, g1_b, s_t, s_b, junk, junk_b)
            outs.append(P.dma("gpsimd", d_xmid[t * 128:(t + 1) * 128, :], x_t[:], reads=[x_b]))


def emit_post(P, f_t, f_b, x_t, x_b, grow, grow_b, s_t, s_b, junk, junk_b):
    P.op("scalar", lambda e: e.activation(out=junk[:], in_=f_t[:], func=AF.Square, accum_out=s_t[:, 12:13]),
         reads=[f_b], writes=[junk_b, s_b])
    P.op("scalar", lambda e: e.activation(out=s_t[:, 13:14], in_=s_t[:, 12:13], func=AF.Sqrt, scale=1.0 / D, bias=EPS),
         reads=[s_b], writes=[s_b])
    P.op("vector", lambda e: e.reciprocal(out=s_t[:, 14:15], in_=s_t[:, 13:14]), reads=[s_b], writes=[s_b])
    P.op("vector", lambda e: e.scalar_tensor_tensor(out=f_t[:], in0=f_t[:], scalar=s_t[:, 14:15], in1=grow[:],
                                                    op0=ALU.mult, op1=ALU.mult), reads=[f_b, s_b, grow_b], writes=[f_b])
    P.op("gpsimd", lambda e: e.tensor_tensor(out=x_t[:], in0=x_t[:], in1=f_t[:], op=ALU.add), reads=[x_b, f_b],
         writes=[x_b])


def emit_ffn(P, C, pss, d_xmid, d_xout, d_cT, d_aw, d_abT, d_abrow, d_npre, d_npost, d_wg, d_wu, d_wd, n_exp,
             d_rw, d_rb, ident, ident_b, outs, in_b=None):
    rd = [in_b] if in_b is not None else []
    zeros, zeros_b = T(P, "f_zeros", [128, 128], F32)
    P.op("gpsimd", lambda e: e.memset(zeros[:], 0.0), writes=[zeros_b])
    cond, cond_b = emit_cond(P, d_cT)
    mod, mod_b = emit_ada_fm(P, cond, cond_b, d_aw, d_abT, 24, 16, pss[0][0], pss[0][1], "f_mod")
    npre, npre_b = T(P, "f_npre", [128, 8], F32)
    P.dma("sync", npre[:], d_npre, writes=[npre_b])
    a2, a2_b = T(P, "f_a2", [128, 8], F32)
    P.op("vector", lambda e: e.scalar_tensor_tensor(out=a2[:], in0=mod[:, 8:16], scalar=1.0, in1=npre[:], op0=ALU.add,
                                                    op1=ALU.mult), reads=[mod_b, npre_b], writes=[a2_b])
    g2, g2_b = emit_ada_bc(P, cond, cond_b, d_aw, d_abrow, 5 * D, D, [pss[0], pss[1]], "f_g2", zeros, zeros_b)
    npost, npost_b = emit_rowbc(P, d_npost, D, "f_npost")
    P.op("vector", lambda e: e.tensor_tensor(out=g2[:], in0=g2[:], in1=npost[:], op=ALU.mult), reads=[g2_b, npost_b],
         writes=[g2_b])
    h2d = P.dram("f_h2T", [128, 8, TOK], BF16).ap()
    h2d_b = Buf("h2d")
    accd = P.dram("f_acc", [TOK, D], F32).ap()
    accd_b = Buf("accd")
    hT = P.sbuf("f_hT", [128, 8, 512], BF16)
    moe = n_exp > 1
    if moe:
        rw, rw_b = T(P, "f_rw", [128, 8, 8], F32)
        P.dma("sync", rw[:], d_rw.rearrange("(k p) e -> p k e", p=128), writes=[rw_b])
        rb, rb_b = emit_rowbc(P, d_rb, 8, "f_rb")
        gw, gw_b = T(P, "f_gw", [128, NT, 8], F32)
        h32 = P.sbuf("f_h32", [128, 8, 512], F32)
    for tc in range(8):
        hb = [[Buf(f"fh{tc}_{t}_{k}") for k in range(8)] for t in range(4)]
        h32b = [[Buf(f"fh32{tc}_{t}_{k}") for k in range(8)] for t in range(4)]
        emit_norm_hT(P, C, d_xmid[tc * 512:(tc + 1) * 512, :], a2, a2_b, mod, mod_b, ident, ident_b, hT, hb,
                     [(pss[2], pss[3]), (pss[4], pss[5])], f"fn{tc}", n_tiles=4,
                     hT32=(h32, h32b) if moe else None, extra_reads=rd)
        allb = [b for t in range(4) for b in hb[t]]
        P.dma("gpsimd", h2d[:, :, tc * 512:(tc + 1) * 512], hT[:], reads=allb, writes=[h2d_b])
        if moe:
            for sub in range(4):
                t = tc * 4 + sub
                pl, pl_b = pss[6 + (t % 2)]
                for kc in range(8):
                    P.op("tensor", lambda e, pl=pl, kc=kc, sub=sub: e.matmul(
                        pl[:, 0:8], lhsT=h32[:, kc, sub * 128:(sub + 1) * 128], rhs=rw[:, kc, :], start=(kc == 0),
                        stop=(kc == 7)), reads=[rw_b, h32b[sub][kc]], writes=[pl_b])
                emit_route(P, pl, pl_b, rb, rb_b, gw, gw_b, t)
    wg = P.sbuf("f_wg", [128, 8, DFF], BF16)
    wu = P.sbuf("f_wu", [128, 8, DFF], BF16)
    wd = P.sbuf("f_wd", [128, NFC, D], BF16)
    hc = [T(P, f"f_hc{i}", [128, 8, 512], BF16) for i in range(2)]
    actT, actT_b = T(P, "f_act", [128, NFC, 512], BF16)
    sg = [T(P, f"f_sg{i}", [128, 512], F32) for i in range(2)]
    at = [T(P, f"f_at{i}", [128, D], F32) for i in range(2)]
    ci = [0]
    for ex in range(n_exp):
        wg_b, wu_b, wd_b = [], [], []
        load_w_bf16(P, C, d_wg[ex], 8, DFF, wg, wg_b, f"f_wg{ex}", col_chunk=176)
        load_w_bf16(P, C, d_wu[ex], 8, DFF, wu, wu_b, f"f_wu{ex}", col_chunk=176)
        load_w_bf16(P, C, d_wd[ex], NFC, D, wd, wd_b, f"f_wd{ex}", col_chunk=64)
        for tc in range(8):
            h_t, h_b = hc[ci[0] % 2]
            ci[0] += 1
            P.dma("sync", h_t[:], h2d[:, :, tc * 512:(tc + 1) * 512], reads=[h2d_b], writes=[h_b])
            for fc in range(NFC):
                fsl = slice(fc * 128, (fc + 1) * 128)
                pg, pg_b = pss[(fc % 2) * 2]
                pu, pu_b = pss[(fc % 2) * 2 + 1]
                s_t, s_b = sg[fc % 2]
                for kc in range(8):
                    P.op("tensor", lambda e, pg=pg, kc=kc, fsl=fsl, h_t=h_t: e.matmul(
                        pg[:], lhsT=wg[:, kc, fsl], rhs=h_t[:, kc, :], start=(kc == 0), stop=(kc == 7)),
                        reads=wg_b + [h_b], writes=[pg_b])
                for kc in range(8):
                    P.op("tensor", lambda e, pu=pu, kc=kc, fsl=fsl, h_t=h_t: e.matmul(
                        pu[:], lhsT=wu[:, kc, fsl], rhs=h_t[:, kc, :], start=(kc == 0), stop=(kc == 7)),
                        reads=wu_b + [h_b], writes=[pu_b])
                P.op("scalar", lambda e, pg=pg, s_t=s_t: e.activation(out=s_t[:], in_=pg[:], func=AF.Silu),
                     reads=[pg_b], writes=[s_b])
                P.op("vector", lambda e, pu=pu, s_t=s_t, fc=fc: e.tensor_tensor(
                    out=actT[:, fc, :], in0=pu[:], in1=s_t[:], op=ALU.mult), reads=[pu_b, s_b], writes=[actT_b])
            for sub in range(4):
                t = tc * 4 + sub
                a_t, a_b = at[t % 2]
                if ex > 0:
                    P.dma("sync", a_t[:], accd[t * 128:(t + 1) * 128, :], reads=[accd_b], writes=[a_b])
                for half in range(2):
                    pf, pf_b = pss[4 + half]
                    for fc in range(NFC):
                        P.op("tensor", lambda e, pf=pf, fc=fc, half=half, sub=sub: e.matmul(
                            pf[:], lhsT=actT[:, fc, sub * 128:(sub + 1) * 128],
                            rhs=wd[:, fc, half * 512:(half + 1) * 512], start=(fc == 0), stop=(fc == NFC - 1)),
                            reads=wd_b + [actT_b], writes=[pf_b])
                    hs = slice(half * 512, (half + 1) * 512)
                    if not moe:
                        P.op("scalar", lambda e, pf=pf, a_t=a_t, hs=hs: e.copy(out=a_t[:, hs], in_=pf[:]),
                             reads=[pf_b], writes=[a_b])
                    elif ex == 0:
                        P.op("scalar", lambda e, pf=pf, a_t=a_t, hs=hs, t=t, ex=ex: e.activation(
                            out=a_t[:, hs], in_=pf[:], func=AF.Copy, scale=gw[:, t, ex:ex + 1]),
                            reads=[pf_b, gw_b], writes=[a_b])
                    else:
                        P.op("vector", lambda e, pf=pf, a_t=a_t, hs=hs, t=t, ex=ex: e.scalar_tensor_tensor(
                            out=a_t[:, hs], in0=pf[:], scalar=gw[:, t, ex:ex + 1], in1=a_t[:, hs], op0=ALU.mult,
                            op1=ALU.add), reads=[pf_b, gw_b, a_b], writes=[a_b])
                P.dma("gpsimd", accd[t * 128:(t + 1) * 128, :], a_t[:], reads=[a_b], writes=[accd_b])
    xt = [T(P, f"f_x{i}", [128, D], F32) for i in range(2)]
    ft = [T(P, f"f_f{i}", [128, D], F32) for i in range(2)]
    st = [T(P, f"f_st{i}", [128, 16], F32) for i in range(2)]
    junk, junk_b = T(P, "f_junk", [128, D], BF16)
    for t in range(NT):
        x_t, x_b = xt[t % 2]
        f_t, f_b = ft[t % 2]
        s_t, s_b = st[t % 2]
        P.dma("sync", x_t[:], d_xmid[t * 128:(t + 1) * 128, :], reads=rd, writes=[x_b])
        P.dma("sync", f_t[:], accd[t * 128:(t + 1) * 128, :], reads=[accd_b], writes=[f_b])
        emit_post(P, f_t, f_b, x_t, x_b, g2, g2_b, s_t, s_b, junk, junk_b)
        outs.append(P.dma("gpsimd", d_xout[t * 128:(t + 1) * 128, :], x_t[:], reads=[x_b]))


def emit_route(P, pl, pl_b, rb, rb_b, gw, gw_b, t):
    lg, lg_b = T(P, f"rt_lg{t}", [128, 40], F32)
    P.op("vector", lambda e: e.tensor_tensor(out=lg[:, 0:8], in0=pl[:, 0:8], in1=rb[:], op=ALU.add),
         reads=[pl_b, rb_b], writes=[lg_b])
    P.op("vector", lambda e: e.tensor_reduce(out=lg[:, 8:9], in_=lg[:, 0:8], axis=AX.X, op=ALU.max), reads=[lg_b],
         writes=[lg_b])
    P.op("vector", lambda e: e.tensor_scalar(out=lg[:, 16:24], in0=lg[:, 0:8], scalar1=lg[:, 8:9], scalar2=None,
                                             op0=ALU.is_equal), reads=[lg_b], writes=[lg_b])
    P.op("vector", lambda e: e.scalar_tensor_tensor(out=lg[:, 24:32], in0=lg[:, 16:24], scalar=-1.0e30, in1=lg[:, 0:8],
                                                    op0=ALU.mult, op1=ALU.add), reads=[lg_b], writes=[lg_b])
    P.op("vector", lambda e: e.tensor_reduce(out=lg[:, 9:10], in_=lg[:, 24:32], axis=AX.X, op=ALU.max), reads=[lg_b],
         writes=[lg_b])
    P.op("vector", lambda e: e.tensor_scalar(out=lg[:, 32:40], in0=lg[:, 24:32], scalar1=lg[:, 9:10], scalar2=None,
                                             op0=ALU.is_equal), reads=[lg_b], writes=[lg_b])
    P.op("vector", lambda e: e.tensor_tensor(out=lg[:, 16:24], in0=lg[:, 16:24], in1=lg[:, 32:40], op=ALU.add),
         reads=[lg_b], writes=[lg_b])
    P.op("vector", lambda e: e.tensor_scalar(out=lg[:, 10:11], in0=lg[:, 8:9], scalar1=-1.0, scalar2=None,
                                             op0=ALU.mult), reads=[lg_b], writes=[lg_b])
    P.op("scalar", lambda e: e.activation(out=lg[:, 24:32], in_=lg[:, 0:8], func=AF.Exp, bias=lg[:, 10:11], scale=1.0),
         reads=[lg_b], writes=[lg_b])
    P.op("vector", lambda e: e.tensor_tensor(out=lg[:, 24:32], in0=lg[:, 24:32], in1=lg[:, 16:24], op=ALU.mult),
         reads=[lg_b], writes=[lg_b])
    P.op("vector", lambda e: e.tensor_reduce(out=lg[:, 11:12], in_=lg[:, 24:32], axis=AX.X, op=ALU.add), reads=[lg_b],
         writes=[lg_b])
    P.op("vector", lambda e: e.reciprocal(out=lg[:, 12:13], in_=lg[:, 11:12]), reads=[lg_b], writes=[lg_b])
    P.op("vector", lambda e: e.tensor_scalar(out=gw[:, t, :], in0=lg[:, 24:32], scalar1=lg[:, 12:13], scalar2=None,
                                             op0=ALU.mult), reads=[lg_b], writes=[gw_b])


GRP4 = [[0, 1, 2, 3], [4, 5, 6, 7]]
GRP8 = [list(range(8))]
KVCH = 6
KVSZ = (NK + NV) // KVCH


class WStage:
    def __init__(self, P, tag, elems=2048):
        self.bufs = [T(P, f"{tag}_ws{i}", [128, elems], F32) for i in range(2)]
        self.i = 0

    def next(self):
        r = self.bufs[self.i % 2]
        self.i += 1
        return r


def load_w(P, C, ws, dW, kch, ncols, wb, wb_b, cast_engs=("gpsimd", "vector")):
    wv = dW.rearrange("(k p) n -> p k n", p=128)
    cc = max(1, 2048 // kch)
    while ncols % cc and ncols % cc < 8:
        cc -= 1
    for ci, c0 in enumerate(range(0, ncols, cc)):
        cn = min(cc, ncols - c0)
        st, st_b = ws.next()
        sv = st[:, 0:kch * cn].rearrange("p (k c) -> p k c", k=kch)
        P.dma(C.ld_eng(), sv, wv[:, :, c0:c0 + cn], writes=[st_b])
        cb = Buf("wc")
        wb_b.append(cb)
        P.op(cast_engs[ci % len(cast_engs)], lambda e, sv=sv, c0=c0, cn=cn: e.tensor_copy(out=wb[:, :, c0:c0 + cn], in_=sv),
             reads=[st_b], writes=[cb])


def ada_fm(P, ws, cond, cond_b, dAW, dABT, j0, nj, ps, ps_b, name):
    res, res_b = T(P, name, [128, nj], F32)
    abt, abt_b = T(P, name + "_ab", [128, nj], F32)
    P.dma("sync", abt[:], dABT[:, j0:j0 + nj], writes=[abt_b])
    awv = dAW.rearrange("(k p) n -> p k n", p=128)
    for c0 in range(0, nj, 2):
        cn = min(2, nj - c0)
        st, st_b = ws.next()
        sv = st[:, 0:8 * cn * 128].rearrange("p (k c) -> p k c", k=8)
        P.dma("sync" if (c0 // 2) % 2 == 0 else "gpsimd", sv, awv[:, :, (j0 + c0) * 128:(j0 + c0 + cn) * 128],
              writes=[st_b])
        for jj in range(cn):
            for kc in range(8):
                P.op("tensor", lambda e, jj=jj, kc=kc, sv=sv, c0=c0: e.matmul(
                    ps[:, c0 + jj:c0 + jj + 1], lhsT=sv[:, kc, jj * 128:(jj + 1) * 128], rhs=cond[:, kc:kc + 1],
                    start=(kc == 0), stop=(kc == 7)), reads=[st_b, cond_b], writes=[ps_b])
    P.op("vector", lambda e: e.tensor_tensor(out=res[:], in0=ps[:, :nj], in1=abt[:], op=ALU.add),
         reads=[ps_b, abt_b], writes=[res_b])
    return res, res_b


def ada_bc(P, ws, cond, cond_b, dAW, dAB_row, col0, ncols, pss2, name):
    res, res_b = T(P, name, [128, ncols], F32)
    crep, crep_b = T(P, name + "_crep", [128, 8, 128], F32)
    P.op("vector", lambda e: e.tensor_copy(out=crep[:], in_=cond[:, 0:8].unsqueeze(2).to_broadcast([128, 8, 128])),
         reads=[cond_b], writes=[crep_b])
    P.dma("sync", res[:], dAB_row[col0:col0 + ncols].partition_broadcast(128), writes=[res_b])
    awv = dAW.rearrange("(k p) n -> p k n", p=128)
    for ci, c0 in enumerate(range(0, ncols, 256)):
        st, st_b = ws.next()
        sv = st[:, 0:2048].rearrange("p (k c) -> p k c", k=8)
        ps, ps_b = pss2[ci % len(pss2)]
        P.dma("sync" if ci % 2 == 0 else "gpsimd", sv, awv[:, :, col0 + c0:col0 + c0 + 256], writes=[st_b])
        for kc in range(8):
            P.op("tensor", lambda e, kc=kc, sv=sv, ps=ps: e.matmul(
                ps[:, :256], lhsT=crep[:, kc, :], rhs=sv[:, kc, :], start=(kc == 0), stop=(kc == 7)),
                reads=[st_b, crep_b], writes=[ps_b])
        P.op("vector", lambda e, ps=ps, c0=c0: e.tensor_tensor(out=res[:, c0:c0 + 256], in0=ps[:, :256],
                                                               in1=res[:, c0:c0 + 256], op=ALU.add),
             reads=[ps_b, res_b], writes=[res_b])
    return res, res_b


class NormBufs:
    def __init__(self, P, tag):
        self.xs = [T(P, f"{tag}_x{i}", [128, D], F32) for i in range(3)]
        self.xn = [T(P, f"{tag}_xn{i}", [128, D], F32) for i in range(2)]
        self.junk = T(P, f"{tag}_junk", [128, D], BF16)
        self.st = [T(P, f"{tag}_st{i}", [128, 4], F32) for i in range(2)]
        self.n = 0


def norm_hT(P, C, nb, dXsrc, n_tiles, a_fm, a_b, sh_fm, sh_b, ident, ident_b, psT, out_fn, rd=()):
    junk, junk_b = nb.junk
    for t in range(n_tiles):
        i = nb.n
        nb.n += 1
        x_t, x_b = nb.xs[i % 3]
        xn_t, xn_b = nb.xn[i % 2]
        s_t, s_b = nb.st[i % 2]
        P.dma(C.ld_eng(), x_t[:], dXsrc[t * 128:(t + 1) * 128, :], reads=list(rd), writes=[x_b])
        P.op("scalar", lambda e, x_t=x_t, s_t=s_t: e.activation(out=junk[:], in_=x_t[:], func=AF.Square,
                                                               accum_out=s_t[:, 0:1]),
             reads=[x_b], writes=[junk_b, s_b])
        P.op("scalar", lambda e, s_t=s_t: e.activation(out=s_t[:, 1:2], in_=s_t[:, 0:1], func=AF.Sqrt,
                                                       scale=1.0 / D, bias=EPS), reads=[s_b], writes=[s_b])
        P.op("vector", lambda e, s_t=s_t: e.reciprocal(out=s_t[:, 2:3], in_=s_t[:, 1:2]), reads=[s_b], writes=[s_b])
        P.op("vector", lambda e, x_t=x_t, xn_t=xn_t, s_t=s_t: e.tensor_scalar(
            out=xn_t[:], in0=x_t[:], scalar1=s_t[:, 2:3], scalar2=None, op0=ALU.mult),
            reads=[x_b, s_b], writes=[xn_b])
        (pa, pa_b), (pb, pb_b) = psT[i % 2]
        for k in range(8):
            pp, pp_b = (pa, pa_b) if k < 4 else (pb, pb_b)
            P.op("tensor", lambda e, k=k, pp=pp, xn_t=xn_t: e.transpose(
                out=pp[:, (k % 4) * 128:(k % 4 + 1) * 128], in_=xn_t[:, k * 128:(k + 1) * 128], identity=ident[:]),
                reads=[xn_b, ident_b], writes=[pp_b])
        for k in range(8):
            pp, pp_b = (pa, pa_b) if k < 4 else (pb, pb_b)
            out_fn(t, k, pp[:, (k % 4) * 128:(k % 4 + 1) * 128], pp_b)


def pe_reset(P):
    t_, b_ = T(P, "pe_rst", [128, 64], BF16)
    ps = P.psum("pe_rst_ps", [128, 64], F32)
    pb = Buf("pe_rst_ps", psum=True)
    P.op("gpsimd", lambda e: e.memset(t_[:], 0.0), writes=[b_])
    for _ in range(2):
        P.op("tensor", lambda e: e.matmul(ps[0:64, :], lhsT=t_[:], rhs=t_[:], start=True, stop=True), reads=[b_],
             writes=[pb])


def end_phase(P, outs, nc):
    P.finish_wait("sync", outs)
    P.emit()
    nc.all_engine_barrier()


WSPEC = [("ada_w", 2048, 6144), ("w_in", 2048, NCOL), ("w_merge", 8192, 1024), ("w_branch", 2048, 1024),
         ("w_o", 2048, 1024), ("ffn_w_gate", 1024, DFF), ("ffn_w_up", 1024, DFF), ("ffn_w_down", DFF, 1024),
         ("moe_w_gate", 8192, DFF), ("moe_w_up", 8192, DFF), ("moe_w_down", 8 * DFF, 1024)]


def phase_weights(nc, need):
    full = {}
    for name, rows, cols in WSPEC:
        if name in need:
            full[name] = din(nc, name, [rows, cols])
    return full


def phase_A(nc, l, d_x, d_cT, W, sm, D_):
    P = Prog(nc)
    C = Ctx(P)
    pss = P.psum_banks()
    ws = WStage(P, "A")
    ident, ident_b = T(P, "ident", [128, 128], F32)
    P.dma("sync", ident[:], D_["ident"], writes=[ident_b])
    dAW = W["ada_w"][l * 1024:(l + 1) * 1024, :]
    dWin = W["w_in"][l * 1024:(l + 1) * 1024, :]
    cond, cond_b = emit_cond(P, d_cT)
    mod, mod_b = ada_fm(P, ws, cond, cond_b, dAW, sm["ada_bT"], 0, 16, pss[0][0], pss[0][1], "modA")
    npre, npre_b = T(P, "npre", [128, 8], F32)
    P.dma("sync", npre[:], sm["npreT"], writes=[npre_b])
    a1, a1_b = T(P, "a1", [128, 8], F32)
    P.op("vector", lambda e: e.scalar_tensor_tensor(out=a1[:], in0=mod[:, 8:16], scalar=1.0, in1=npre[:],
                                                    op0=ALU.add, op1=ALU.mult), reads=[mod_b, npre_b], writes=[a1_b])
    hT = P.sbuf("hT", [128, 8, TOK], BF16)
    hT_bufs = [[Buf(f"hT{t}_{k}") for k in range(8)] for t in range(NT)]
    wb = P.sbuf("w_in_bf", [128, 8, NCOL], BF16)
    wb_b = []
    load_w(P, C, ws, dWin, 8, NCOL, wb, wb_b)
    nb = NormBufs(P, "nA")

    def out_fn(t, k, src, pp_b):
        P.op("scalar", lambda e: e.activation(out=hT[:, k, t * 128:(t + 1) * 128], in_=src, func=AF.Identity,
                                              scale=a1[:, k:k + 1], bias=mod[:, k:k + 1]),
             reads=[pp_b, a1_b, mod_b], writes=[hT_bufs[t][k]])
    norm_hT(P, C, nb, d_x, NT, a1, a1_b, mod, mod_b, ident, ident_b, [(pss[0], pss[1]), (pss[2], pss[3])], out_fn)
    outs = []

    def store(dst, src, reads):
        outs.append(P.dma("gpsimd", dst, src, reads=reads))
    for c in range(8):
        store(D_["hT"][:, :, c * 512:(c + 1) * 512], hT[:, :, c * 512:(c + 1) * 512],
              [b for t in range(c * 4, c * 4 + 4) for b in hT_bufs[t]])
    scale = 32 ** -0.5
    pj = [pss[4], pss[5], pss[6], pss[7]]
    pji = [0]

    def next_ps():
        r = pj[pji[0] % 4]
        pji[0] += 1
        return r
    ostg_f = [T(P, f"ostg_f{i}", [128, 512], F32) for i in range(4)]
    ostg_b = [T(P, f"ostg_b{i}", [128, 512], BF16) for i in range(3)]
    cnt = [0, 0]
    kvK = D_["kvloc"][0:NK].rearrange("(m p t) -> m p t", m=2, p=128)
    kvV = D_["kvloc"][NK:NK + NV].rearrange("(h p t d) -> h p t d", h=4, p=128, t=32)
    o_fm = D_["fmT"]
    for tc in range(8):
        tsl = slice(tc * 512, (tc + 1) * 512)

        def fm(col0, tc=tc, tsl=tsl):
            ps, ps_b = next_ps()
            for kc in range(8):
                P.op("tensor", lambda e, kc=kc, ps=ps, col0=col0, tsl=tsl: e.matmul(
                    ps[:], lhsT=wb[:, kc, col0:col0 + 128], rhs=hT[:, kc, tsl], start=(kc == 0), stop=(kc == 7)),
                    reads=wb_b + [hT_bufs[tc * 4 + i][kc] for i in range(4)], writes=[ps_b])
            return ps, ps_b
        for g in range(4):
            ps, ps_b = fm(g * 128)
            st, st_b = ostg_b[cnt[0] % 3]
            cnt[0] += 1
            sc = scale if g < 2 else 1.0
            P.op("scalar", lambda e, ps=ps, st=st, sc=sc: e.activation(out=st[:], in_=ps[:], func=AF.Copy, scale=sc),
                 reads=[ps_b], writes=[st_b])
            if g < 2:
                store(D_["qkT"][g, :, tsl], st[:], [st_b])
            else:
                store(kvK[g - 2, :, tsl], st[:], [st_b])
        for j in range(4):
            ps, ps_b = fm(768 + j * 128)
            st, st_b = ostg_f[cnt[1] % 4]
            cnt[1] += 1
            copy_op(P, C.ev_eng(), st[:], ps[:], [ps_b], [st_b])
            store(o_fm[j if j < 2 else 8 + j, :, tsl], st[:], [st_b])
        for j in range(2):
            psc, psc_b = fm(1280 + j * 128)
            psx, psx_b = fm(1536 + j * 128)
            tmp, tmp_b = ostg_f[cnt[1] % 4]
            cnt[1] += 1
            st, st_b = ostg_f[cnt[1] % 4]
            cnt[1] += 1
            P.op("scalar", lambda e, psc=psc, tmp=tmp: e.copy(out=tmp[:], in_=psc[:]), reads=[psc_b], writes=[tmp_b])
            P.op("vector", lambda e, psx=psx, tmp=tmp, st=st: e.tensor_tensor(out=st[:], in0=psx[:], in1=tmp[:],
                                                                             op=ALU.mult),
                 reads=[psx_b, tmp_b], writes=[st_b])
            store(o_fm[2 + j, :, tsl], st[:], [st_b])
        for j in range(6):
            ps, ps_b = fm(1792 + j * 128)
            st, st_b = ostg_f[cnt[1] % 4]
            cnt[1] += 1
            copy_op(P, C.ev_eng(), st[:], ps[:], [ps_b], [st_b])
            store(o_fm[4 + j, :, tsl], st[:], [st_b])
    vst = [T(P, f"vst{i}", [128, 4, 65], BF16) for i in range(2)]
    for i in range(2):
        P.op("gpsimd", lambda e, i=i: e.memset(vst[i][0][:], 1.0), writes=[vst[i][1]])
    zst = [T(P, f"zst{i}", [128, 272], F32) for i in range(2)]
    for t in range(NT):
        ps, ps_b = next_ps()
        for kc in range(8):
            P.op("tensor", lambda e, kc=kc, ps=ps, t=t: e.matmul(
                ps[:, 0:256], lhsT=hT[:, kc, t * 128:(t + 1) * 128], rhs=wb[:, kc, 512:768], start=(kc == 0),
                stop=(kc == 7)), reads=wb_b + [hT_bufs[t][kc]], writes=[ps_b])
        v_t, v_b = vst[t % 2]
        P.op("vector", lambda e, ps=ps, v_t=v_t: e.tensor_copy(
            out=v_t[:, :, 0:64], in_=ps[:, 0:256].rearrange("p (h d) -> p h d", h=4)), reads=[ps_b], writes=[v_b])
        store(kvV[:, :, t, :].rearrange("h p d -> p h d"), v_t[:], [v_b])
        ps, ps_b = next_ps()
        for kc in range(8):
            P.op("tensor", lambda e, kc=kc, ps=ps, t=t: e.matmul(
                ps[:, 0:272], lhsT=hT[:, kc, t * 128:(t + 1) * 128], rhs=wb[:, kc, 2560:2832], start=(kc == 0),
                stop=(kc == 7)), reads=wb_b + [hT_bufs[t][kc]], writes=[ps_b])
        z_t, z_b = zst[t % 2]
        P.op("scalar", lambda e, ps=ps, z_t=z_t: e.copy(out=z_t[:], in_=ps[:, 0:272]), reads=[ps_b], writes=[z_b])
        store(D_["dz"][t * 128:(t + 1) * 128, :], z_t[:, 0:256], [z_b])
        store(D_["dbg"][t * 128:(t + 1) * 128, :], z_t[:, 256:272], [z_b])
    fin = P.op("sync", lambda e: None)
    fin.deps = list(outs)
    e1 = P.dma("sync", D_["edge_loc"][:, :, 0:HAL], o_fm[0:10, :, 0:HAL])
    e2 = P.dma("sync", D_["edge_loc"][:, :, HAL:2 * HAL], o_fm[0:10, :, TOK - HAL:TOK])
    colls = []
    for c in range(KVCH):
        cc = P.coll("AllGather", GRP4, D_["kvloc"][c * KVSZ:(c + 1) * KVSZ].rearrange("(a n) -> a n", a=1),
                    D_["kvall"][c])
        cc.deps.extend(outs)
        colls.append(cc)
    c2 = P.coll("AllGather", GRP4, D_["edge_loc"].rearrange("j p c -> (j p) c"),
                D_["edge_all"].rearrange("r j p c -> (r j p) c"))
    c2.deps.extend([e1, e2])
    c1 = colls[-1]
    outs.extend(colls)
    end_phase(P, outs + [c1, c2], nc)


def phase_ATT(nc, l, sm, D_, lambda_init):
    P = Prog(nc)
    C = Ctx(P)
    pss = P.psum_banks()
    ident, ident_b = T(P, "ident", [128, 128], F32)
    P.dma("sync", ident[:], D_["ident"], writes=[ident_b])
    outs = []
    emit_att(P, C, pss, D_["qkT"], D_["kvall"], D_["oaT"], sm["dlam"], sm["subln"],
             (D_["qaug"], D_["kaug"], D_["biasF"], D_["dtab"], D_["identb"]), ident, ident_b, lambda_init, outs)
    end_phase(P, outs, nc)


def phase_B(nc, l, sm, D_):
    P = Prog(nc)
    C = Ctx(P)
    pss = P.psum_banks()
    ident, ident_b = T(P, "ident", [128, 128], F32)
    P.dma("sync", ident[:], D_["ident"], writes=[ident_b])
    outs = []
    tabs = (D_["bmask"], D_["bcorr"], D_["binvw"], sm["poolw"], sm["pscale"], sm["sconvT"], sm["dconvT"],
            sm["alog"], sm["dtb"])
    emit_b(P, C, pss, D_["fmT"], D_["edge_all"], D_["dbg"], D_["obT"], D_["ocT"], D_["dsend"], tabs, ident, ident_b, outs)
    colls = []
    for c in range(16):
        cc = P.coll("AllGather", GRP4, D_["dsend"][c].rearrange("(a h) t f -> a (h t f)", a=1),
                    D_["dall"][c].rearrange("r h t f -> r (h t f)"))
        cc.deps.extend(outs)
        colls.append(cc)
    cps = []
    for r in range(4):
        def cp(e, r=r):
            rk = P.rank_val(e, 0, "scalar")
            return e.dma_start(out=D_["dloc"][r * TOK:(r + 1) * TOK, :].rearrange("(c t) f -> c (t f)", c=16),
                               in_=D_["dall"][:, r, bass.ds(rk, 1), :, :].rearrange("c a t f -> c (a t f)"))
        o = P.dyn_dma("scalar", cp)
        o.deps.append(colls[-1])
        cps.append(o)
    end_phase(P, outs + colls + cps, nc)


def phase_DELTA(nc, l, D_):
    P = Prog(nc)
    C = Ctx(P)
    pss = P.psum_banks()
    ident, ident_b = T(P, "ident", [128, 128], F32)
    P.dma("sync", ident[:], D_["ident"], writes=[ident_b])
    outs = []
    emit_delta(P, C, pss, D_["dloc"], lambda ch, tok0: D_["do"][tok0 // 1024, ch, tok0 % 1024:tok0 % 1024 + 512, :], D_["dconst"], ident, ident_b, outs)
    colls = []
    for c in range(16):
        cc = P.coll("AllGather", GRP4, D_["do"][c].rearrange("(a d) t f -> a (d t f)", a=1),
                    D_["doall"][c // 4, c % 4].rearrange("h d t f -> h (d t f)"))
        cc.deps.extend(outs)
        colls.append(cc)
    cps = []
    for j in range(4):
        def cp(e, j=j):
            rk = P.rank_val(e, 0, "scalar")
            return e.dma_start(out=D_["dloc_o"][:, :, j * 1024:(j + 1) * 1024, :].rearrange("h d t f -> (h d) (t f)"),
                               in_=D_["doall"][bass.ds(rk, 1), j, :, :, :, :].rearrange("a h d t f -> (a h d) (t f)"))
        o = P.dyn_dma("scalar", cp)
        o.deps.append(colls[-1])
        cps.append(o)
    end_phase(P, outs + colls + cps, nc)


def phase_C1a(nc, l, W, sm, D_):
    P = Prog(nc)
    C = Ctx(P)
    pss = P.psum_banks()
    ws = WStage(P, "C1")
    ident, ident_b = T(P, "ident", [128, 128], F32)
    P.dma("sync", ident[:], D_["ident"], writes=[ident_b])
    outs = []
    dnorm, dnorm_b = emit_rowbc(P, sm["dnorm"], 64, "c1dnorm")
    STG = int(os.environ.get("C1STAGE", "9"))
    if STG == 1:
        return end_phase(P, outs, nc)
    wms = [P.sbuf(f"c1_wm{i}", [128, 8, D], BF16) for i in range(4)]
    wm_b = []
    for i in range(4):
        load_w(P, C, ws, W["w_merge"][(l * 4 + i) * 1024:(l * 4 + i + 1) * 1024, :], 8, D, wms[i], wm_b)
    wbrs = [P.sbuf(f"c1_wbr{i}", [128, 2, D], BF16) for i in range(4)]
    wbr_b = []
    for i in range(4):
        load_w(P, C, ws, W["w_branch"][(l * 4 + i) * 256:(l * 4 + i + 1) * 256, :], 2, D, wbrs[i], wbr_b)
    bm, bm_b = T(P, "c1_bm", [128, 4, 8], F32)
    P.dma("sync", bm[:], sm["bmergeT"], writes=[bm_b])
    if STG == 2:
        return end_phase(P, outs, nc)
    h_t, h_b = T(P, "c1_hT", [128, 8, 512], BF16)
    b_t, b_b = T(P, "c1_br", [128, 4, 2, 512], BF16)
    m_t, m_b = T(P, "c1_mT", [128, 8, 512], BF16)
    gate = [T(P, f"c1_gate{i}", [128, 512], F32) for i in range(2)]
    acc = [T(P, f"c1_acc{i}", [128, 512], F32) for i in range(2)]
    odt = [T(P, f"c1_od{i}", [128, 4, 2, 64], F32) for i in range(2)]
    dzt = [T(P, f"c1_dz{i}", [128, 256], F32) for i in range(2)]
    ods = [T(P, f"c1_ods{i}", [128, 256], F32) for i in range(2)]
    st = [T(P, f"c1_st{i}", [128, 16], F32) for i in range(2)]
    gi = [0]
    hv = lambda ap: ap.rearrange("p (h f) -> p h f", h=4)
    for tc in range(8):
        tsl = slice(tc * 512, (tc + 1) * 512)
        P.dma("sync", h_t[:], D_["hT"][:, :, tsl], writes=[h_b])
        for i, dsrc in enumerate((D_["oaT"], D_["obT"], D_["ocT"])):
            P.dma("gpsimd", b_t[:, i], dsrc[:, :, tsl].rearrange("c p t -> p c t"), writes=[b_b])
        for sub in (range(4) if STG != 8 else []):
            t = tc * 4 + sub
            o_t, o_b = odt[t % 2]
            z_t, z_b = dzt[t % 2]
            s_t, s_b = st[t % 2]
            d_t, d_b = ods[t % 2]
            P.dma("sync", o_t[:], D_["dloc_o"][:, :, t * 128:(t + 1) * 128, :].rearrange("h d t f -> t h d f"),
                  writes=[o_b])
            P.dma("gpsimd", z_t[:], D_["dz"][t * 128:(t + 1) * 128, :], writes=[z_b])
            sq = o_t[:].rearrange("p h d f -> p (h d f)")[:, 0:256]
            P.op("vector", lambda e, o_t=o_t, d_t=d_t: e.tensor_tensor(out=hv(d_t[:]), in0=o_t[:, :, 0, :],
                                                                       in1=o_t[:, :, 1, :], op=ALU.add),
                 reads=[o_b], writes=[d_b])
            P.op("gpsimd", lambda e, sq=sq, d_t=d_t: e.tensor_tensor(out=sq, in0=d_t[:], in1=d_t[:], op=ALU.mult),
                 reads=[d_b, o_b], writes=[o_b])
            P.op("vector", lambda e, sq=sq, s_t=s_t: e.tensor_reduce(out=s_t[:, 0:4], in_=hv(sq), axis=AX.X, op=ALU.add),
                 reads=[o_b], writes=[s_b])
            P.op("scalar", lambda e, s_t=s_t: e.activation(out=s_t[:, 4:8], in_=s_t[:, 0:4], func=AF.Sqrt,
                                                           scale=1.0 / 64, bias=EPS), reads=[s_b], writes=[s_b])
            P.op("vector", lambda e, s_t=s_t: e.reciprocal(out=s_t[:, 8:12], in_=s_t[:, 4:8]), reads=[s_b], writes=[s_b])
            P.op("scalar", lambda e, z_t=z_t: e.activation(out=z_t[:], in_=z_t[:], func=AF.Silu), reads=[z_b],
                 writes=[z_b])
            P.op("vector", lambda e, d_t=d_t, s_t=s_t: e.tensor_tensor(
                out=hv(d_t[:]), in0=hv(d_t[:]), in1=s_t[:, 8:12].unsqueeze(2).to_broadcast([128, 4, 64]), op=ALU.mult),
                reads=[d_b, s_b], writes=[d_b])
            P.op("gpsimd", lambda e, d_t=d_t: e.tensor_tensor(
                out=hv(d_t[:]), in0=hv(d_t[:]), in1=dnorm[:].unsqueeze(1).to_broadcast([128, 4, 64]), op=ALU.mult),
                reads=[d_b, dnorm_b], writes=[d_b])
            P.op("vector", lambda e, d_t=d_t, z_t=z_t: e.tensor_tensor(out=d_t[:], in0=d_t[:], in1=z_t[:], op=ALU.mult),
                 reads=[d_b, z_b], writes=[d_b])
            pp, pp_b = pss[2 + (t % 2)]
            for c2 in range(2):
                P.op("tensor", lambda e, pp=pp, d_t=d_t, c2=c2: e.transpose(
                    out=pp[:, c2 * 128:(c2 + 1) * 128], in_=d_t[:, c2 * 128:(c2 + 1) * 128], identity=ident[:]),
                    reads=[d_b, ident_b], writes=[pp_b])
            copy_op(P, "scalar", b_t[:, 3, :, sub * 128:(sub + 1) * 128],
                    pp[:, 0:256].rearrange("p (c t) -> p c t", c=2), [pp_b], [b_b])
        if STG == 3:
            continue
        for oc in range(8):
            osl = slice(oc * 128, (oc + 1) * 128)
            a_t, a_b = acc[oc % 2]
            for i in range(4):
                pg, pg_b = pss[4 + (gi[0] % 2)]
                pb_, pb_b = pss[6 + (gi[0] % 2)]
                g_t, g_b = gate[gi[0] % 2]
                gi[0] += 1
                for kc in range(8):
                    P.op("tensor", lambda e, pg=pg, i=i, kc=kc, osl=osl: e.matmul(
                        pg[:], lhsT=wms[i][:, kc, osl], rhs=h_t[:, kc, :], start=(kc == 0), stop=(kc == 7)),
                        reads=wm_b + [h_b], writes=[pg_b])
                if STG in (7, 8):
                    copy_op(P, "scalar", g_t[:], pg[:], [pg_b], [g_b])
                    continue
                if STG == 5:
                    P.op("scalar", lambda e, pg=pg, g_t=g_t, i=i, oc=oc: e.activation(
                        out=g_t[:], in_=pg[:], func=AF.Sigmoid, bias=bm[:, i, oc:oc + 1], scale=1.0),
                        reads=[pg_b, bm_b], writes=[g_b])
                    continue
                for c2 in range(2):
                    P.op("tensor", lambda e, pb_=pb_, i=i, c2=c2, osl=osl: e.matmul(
                        pb_[:], lhsT=wbrs[i][:, c2, osl], rhs=b_t[:, i, c2, :], start=(c2 == 0), stop=(c2 == 1)),
                        reads=wbr_b + [b_b], writes=[pb_b])
                P.op("scalar", lambda e, pg=pg, g_t=g_t, i=i, oc=oc: e.activation(
                    out=g_t[:], in_=pg[:], func=AF.Sigmoid, bias=bm[:, i, oc:oc + 1], scale=1.0),
                    reads=[pg_b, bm_b], writes=[g_b])
                if STG == 6:
                    P.op("vector", lambda e, pb_=pb_, g_t=g_t, a_t=a_t: e.tensor_tensor(
                        out=a_t[:], in0=pb_[:], in1=g_t[:], op=ALU.mult), reads=[pb_b, g_b], writes=[a_b])
                    continue
                if i == 0:
                    P.op("vector", lambda e, pb_=pb_, g_t=g_t, a_t=a_t: e.tensor_tensor(
                        out=a_t[:], in0=pb_[:], in1=g_t[:], op=ALU.mult), reads=[pb_b, g_b], writes=[a_b])
                else:
                    P.op("vector", lambda e, pb_=pb_, g_t=g_t: e.tensor_tensor(
                        out=g_t[:], in0=pb_[:], in1=g_t[:], op=ALU.mult), reads=[pb_b, g_b], writes=[g_b])
                    if i < 3:
                        P.op("gpsimd", lambda e, g_t=g_t, a_t=a_t: e.tensor_tensor(
                            out=a_t[:], in0=a_t[:], in1=g_t[:], op=ALU.add), reads=[a_b, g_b], writes=[a_b])
                    else:
                        P.op("gpsimd", lambda e, g_t=g_t, a_t=a_t, oc=oc: e.tensor_tensor(
                            out=m_t[:, oc, :], in0=a_t[:], in1=g_t[:], op=ALU.add), reads=[a_b, g_b], writes=[m_b])
        outs.append(P.dma("gpsimd", D_["mT"][:, :, tsl], m_t[:], reads=[m_b]))
    end_phase(P, outs, nc)


def phase_C1b(nc, l, d_x, d_cT, W, sm, D_):
    P = Prog(nc)
    C = Ctx(P)
    pss = P.psum_banks()
    ws = WStage(P, "C1b")
    outs = []
    dAW = W["ada_w"][l * 1024:(l + 1) * 1024, :]
    cond, cond_b = emit_cond(P, d_cT)
    g1, g1_b = ada_bc(P, ws, cond, cond_b, dAW, sm["ada_b"], 2 * D, D, [pss[0], pss[1]], "c1g1")
    npost, npost_b = emit_rowbc(P, sm["npost_mix"], D, "c1npost")
    P.op("vector", lambda e: e.tensor_tensor(out=g1[:], in0=g1[:], in1=npost[:], op=ALU.mult), reads=[g1_b, npost_b],
         writes=[g1_b])
    wo = P.sbuf("c1_wo", [128, 8, D], BF16)
    wo_b = []
    load_w(P, C, ws, W["w_o"][l * 1024:(l + 1) * 1024, :], 8, D, wo, wo_b)
    mts = [T(P, f"c1_mT{i}", [128, 8, 512], BF16) for i in range(2)]
    xt = [T(P, f"c1_x{i}", [128, D], F32) for i in range(2)]
    ft = [T(P, f"c1_f{i}", [128, D], F32) for i in range(2)]
    st = [T(P, f"c1_st{i}", [128, 16], F32) for i in range(2)]
    junk, junk_b = T(P, "c1_junk", [128, D], BF16)
    for tc in range(8):
        m_t, m_b = mts[tc % 2]
        P.dma("sync", m_t[:], D_["mT"][:, :, tc * 512:(tc + 1) * 512], writes=[m_b])
        for sub in range(4):
            t = tc * 4 + sub
            x_t, x_b = xt[t % 2]
            f_t, f_b = ft[t % 2]
            s_t, s_b = st[t % 2]
            P.dma("sync", x_t[:], d_x[t * 128:(t + 1) * 128, :], writes=[x_b])
            for half in range(2):
                pf, pf_b = pss[2 + half]
                for kc in range(8):
                    P.op("tensor", lambda e, pf=pf, kc=kc, half=half, sub=sub, m_t=m_t: e.matmul(
                        pf[:], lhsT=m_t[:, kc, sub * 128:(sub + 1) * 128], rhs=wo[:, kc, half * 512:(half + 1) * 512],
                        start=(kc == 0), stop=(kc == 7)), reads=wo_b + [m_b], writes=[pf_b])
                P.op("scalar", lambda e, pf=pf, half=half, f_t=f_t: e.copy(out=f_t[:, half * 512:(half + 1) * 512],
                                                                          in_=pf[:]), reads=[pf_b], writes=[f_b])
            emit_post(P, f_t, f_b, x_t, x_b, g1, g1_b, s_t, s_b, junk, junk_b)
            outs.append(P.dma("gpsimd", D_["xmid"][t * 128:(t + 1) * 128, :], x_t[:], reads=[x_b]))
    end_phase(P, outs, nc)


def phase_F1(nc, l, d_cT, W, sm, D_, moe):
    P = Prog(nc)
    C = Ctx(P)
    pss = P.psum_banks()
    ws = WStage(P, "F1")
    ident, ident_b = T(P, "ident", [128, 128], F32)
    P.dma("sync", ident[:], D_["ident"], writes=[ident_b])
    outs = []
    dAW = W["ada_w"][l * 1024:(l + 1) * 1024, :]
    cond, cond_b = emit_cond(P, d_cT)
    mod, mod_b = ada_fm(P, ws, cond, cond_b, dAW, sm["ada_bT"], 24, 16, pss[0][0], pss[0][1], "f_mod")
    npre, npre_b = T(P, "f_npre", [128, 8], F32)
    P.dma("sync", npre[:], sm["npre_ffnT"], writes=[npre_b])
    a2, a2_b = T(P, "f_a2", [128, 8], F32)
    P.op("vector", lambda e: e.scalar_tensor_tensor(out=a2[:], in0=mod[:, 8:16], scalar=1.0, in1=npre[:], op0=ALU.add,
                                                    op1=ALU.mult), reads=[mod_b, npre_b], writes=[a2_b])
    nb = NormBufs(P, "nF")
    hTs = [P.sbuf(f"f_hT{i}", [128, 8, 512], BF16) for i in range(2)]
    if moe:
        rw, rw_b = T(P, "f_rw", [128, 8, 8], F32)
        P.dma("sync", rw[:], sm["router_w"].rearrange("(k p) e -> p k e", p=128), writes=[rw_b])
        rb, rb_b = emit_rowbc(P, sm["router_b"], 8, "f_rb")
        gw, gw_b = T(P, "f_gw", [128, NT, 8], F32)
        h32s = [P.sbuf(f"f_h32{i}", [128, 8, 512], F32) for i in range(2)]
    for tc in range(8):
        hT = hTs[tc % 2]
        hb = [[Buf(f"fh{tc}_{t}_{k}") for k in range(8)] for t in range(4)]
        if moe:
            h32 = h32s[tc % 2]
            h32b = [[Buf(f"fh32{tc}_{t}_{k}") for k in range(8)] for t in range(4)]

            def out_fn(t, k, src, pp_b, hT=hT, hb=hb, h32=h32, h32b=h32b):
                P.op("scalar", lambda e: e.activation(out=h32[:, k, t * 128:(t + 1) * 128], in_=src, func=AF.Identity,
                                                      scale=a2[:, k:k + 1], bias=mod[:, k:k + 1]),
                     reads=[pp_b, a2_b, mod_b], writes=[h32b[t][k]])
                P.op("gpsimd", lambda e: e.tensor_copy(out=hT[:, k, t * 128:(t + 1) * 128],
                                                       in_=h32[:, k, t * 128:(t + 1) * 128]),
                     reads=[h32b[t][k]], writes=[hb[t][k]])
        else:
            def out_fn(t, k, src, pp_b, hT=hT, hb=hb):
                P.op("scalar", lambda e: e.activation(out=hT[:, k, t * 128:(t + 1) * 128], in_=src, func=AF.Identity,
                                                      scale=a2[:, k:k + 1], bias=mod[:, k:k + 1]),
                     reads=[pp_b, a2_b, mod_b], writes=[hb[t][k]])
        norm_hT(P, C, nb, D_["xmid"][tc * 512:(tc + 1) * 512, :], 4, a2, a2_b, mod, mod_b, ident, ident_b,
                [(pss[2], pss[3]), (pss[4], pss[5])], out_fn)
        allb = [b for t in range(4) for b in hb[t]]
        outs.append(P.dma("gpsimd", D_["h2T"][:, :, tc * 512:(tc + 1) * 512], hT[:], reads=allb))
        if moe:
            for sub in range(4):
                t = tc * 4 + sub
                pl, pl_b = pss[6 + (t % 2)]
                for kc in range(8):
                    P.op("tensor", lambda e, pl=pl, kc=kc, sub=sub, h32=h32: e.matmul(
                        pl[:, 0:8], lhsT=h32[:, kc, sub * 128:(sub + 1) * 128], rhs=rw[:, kc, :], start=(kc == 0),
                        stop=(kc == 7)), reads=[rw_b, h32b[sub][kc]], writes=[pl_b])
                emit_route(P, pl, pl_b, rb, rb_b, gw, gw_b, t)
    if moe:
        outs.append(P.dma("sync", D_["gwd"], gw[:], reads=[gw_b]))
    end_phase(P, outs, nc)


def phase_F2(nc, l, W, D_, moe):
    P = Prog(nc)
    C = Ctx(P)
    pss = P.psum_banks()
    ws = WStage(P, "F2")
    outs = []
    n_exp = 8 if moe else 1
    HF = DFF // 2
    NH = HF // 128
    wg = P.sbuf("f_wg", [128, 8, HF], BF16)
    wu = P.sbuf("f_wu", [128, 8, HF], BF16)
    wd = P.sbuf("f_wd", [128, NH, D], BF16)
    h_t, h_b = T(P, "f_hc", [128, 8, 512], BF16)
    actT, actT_b = T(P, "f_act", [128, NH, 512], BF16)
    sg = [T(P, f"f_sg{i}", [128, 512], F32) for i in range(2)]
    at = [T(P, f"f_at{i}", [128, D], F32) for i in range(2)]
    if moe:
        gw, gw_b = T(P, "f_gw", [128, NT, 8], F32)
        P.dma("sync", gw[:], D_["gwd"], writes=[gw_b])
    last_store = {}
    first = True
    for ex in range(n_exp):
        for hf in range(2):
            wg_b, wu_b, wd_b = [], [], []
            if moe:
                dg = W["moe_w_gate"][ex * 1024:(ex + 1) * 1024, hf * HF:(hf + 1) * HF]
                du = W["moe_w_up"][ex * 1024:(ex + 1) * 1024, hf * HF:(hf + 1) * HF]
                dd = W["moe_w_down"][ex * DFF + hf * HF:ex * DFF + (hf + 1) * HF, :]
            else:
                dg = W["ffn_w_gate"][:, hf * HF:(hf + 1) * HF]
                du = W["ffn_w_up"][:, hf * HF:(hf + 1) * HF]
                dd = W["ffn_w_down"][hf * HF:(hf + 1) * HF, :]
            load_w(P, C, ws, dg, 8, HF, wg, wg_b)
            load_w(P, C, ws, du, 8, HF, wu, wu_b)
            load_w(P, C, ws, dd, NH, D, wd, wd_b)
            for tc in range(8):
                P.dma("sync", h_t[:], D_["h2T"][:, :, tc * 512:(tc + 1) * 512], writes=[h_b])
                for fc in range(NH):
                    fsl = slice(fc * 128, (fc + 1) * 128)
                    pg, pg_b = pss[(fc % 2) * 2]
                    pu, pu_b = pss[(fc % 2) * 2 + 1]
                    s_t, s_b = sg[fc % 2]
                    for kc in range(8):
                        P.op("tensor", lambda e, pg=pg, kc=kc, fsl=fsl: e.matmul(
                            pg[:], lhsT=wg[:, kc, fsl], rhs=h_t[:, kc, :], start=(kc == 0), stop=(kc == 7)),
                            reads=wg_b + [h_b], writes=[pg_b])
                    for kc in range(8):
                        P.op("tensor", lambda e, pu=pu, kc=kc, fsl=fsl: e.matmul(
                            pu[:], lhsT=wu[:, kc, fsl], rhs=h_t[:, kc, :], start=(kc == 0), stop=(kc == 7)),
                            reads=wu_b + [h_b], writes=[pu_b])
                    P.op("scalar", lambda e, pg=pg, s_t=s_t: e.activation(out=s_t[:], in_=pg[:], func=AF.Silu),
                         reads=[pg_b], writes=[s_b])
                    P.op("vector", lambda e, pu=pu, s_t=s_t, fc=fc: e.tensor_tensor(
                        out=actT[:, fc, :], in0=pu[:], in1=s_t[:], op=ALU.mult), reads=[pu_b, s_b], writes=[actT_b])
                for sub in range(4):
                    t = tc * 4 + sub
                    a_t, a_b = at[t % 2]
                    if not first:
                        ld = P.dma("sync", a_t[:], D_["acc"][t * 128:(t + 1) * 128, :], writes=[a_b])
                        ld.deps.append(last_store[t])
                    for half in range(2):
                        pf, pf_b = pss[4 + half]
                        for fc in range(NH):
                            P.op("tensor", lambda e, pf=pf, fc=fc, half=half, sub=sub: e.matmul(
                                pf[:], lhsT=actT[:, fc, sub * 128:(sub + 1) * 128],
                                rhs=wd[:, fc, half * 512:(half + 1) * 512], start=(fc == 0), stop=(fc == NH - 1)),
                                reads=wd_b + [actT_b], writes=[pf_b])
                        hs = slice(half * 512, (half + 1) * 512)
                        if first and not moe:
                            P.op("scalar", lambda e, pf=pf, a_t=a_t, hs=hs: e.copy(out=a_t[:, hs], in_=pf[:]),
                                 reads=[pf_b], writes=[a_b])
                        elif first:
                            P.op("scalar", lambda e, pf=pf, a_t=a_t, hs=hs, t=t, ex=ex: e.activation(
                                out=a_t[:, hs], in_=pf[:], func=AF.Copy, scale=gw[:, t, ex:ex + 1]),
                                reads=[pf_b, gw_b], writes=[a_b])
                        elif not moe:
                            P.op("vector", lambda e, pf=pf, a_t=a_t, hs=hs: e.tensor_tensor(
                                out=a_t[:, hs], in0=pf[:], in1=a_t[:, hs], op=ALU.add), reads=[pf_b, a_b], writes=[a_b])
                        else:
                            P.op("vector", lambda e, pf=pf, a_t=a_t, hs=hs, t=t, ex=ex: e.scalar_tensor_tensor(
                                out=a_t[:, hs], in0=pf[:], scalar=gw[:, t, ex:ex + 1], in1=a_t[:, hs], op0=ALU.mult,
                                op1=ALU.add), reads=[pf_b, gw_b, a_b], writes=[a_b])
                    last_store[t] = P.dma("gpsimd", D_["acc"][t * 128:(t + 1) * 128, :], a_t[:], reads=[a_b])
            first = False
    outs.extend(last_store.values())
    end_phase(P, outs, nc)


def phase_F3(nc, l, d_cT, d_out, W, sm, D_, dbg_g=None):
    P = Prog(nc)
    C = Ctx(P)
    pss = P.psum_banks()
    ws = WStage(P, "F3")
    outs = []
    dAW = W["ada_w"][l * 1024:(l + 1) * 1024, :]
    cond, cond_b = emit_cond(P, d_cT)
    g2, g2_b = ada_bc(P, ws, cond, cond_b, dAW, sm["ada_b"], 5 * D, D, [pss[6], pss[7]], "f_g2")
    npost, npost_b = emit_rowbc(P, sm["npost_ffn"], D, "f_npost")
    P.op("vector", lambda e: e.tensor_tensor(out=g2[:], in0=g2[:], in1=npost[:], op=ALU.mult), reads=[g2_b, npost_b],
         writes=[g2_b])
    if dbg_g is not None:
        outs.append(P.dma("sync", dbg_g, g2[:], reads=[g2_b]))
    xt = [T(P, f"f_x{i}", [128, D], F32) for i in range(3)]
    ft = [T(P, f"f_f{i}", [128, D], F32) for i in range(3)]
    st = [T(P, f"f_st{i}", [128, 16], F32) for i in range(3)]
    junk, junk_b = T(P, "f_junk", [128, D], BF16)
    for t in range(NT):
        x_t, x_b = xt[t % 3]
        f_t, f_b = ft[t % 3]
        s_t, s_b = st[t % 3]
        P.dma("sync", x_t[:], D_["xmid"][t * 128:(t + 1) * 128, :], writes=[x_b])
        P.dma("sync", f_t[:], D_["acc"][t * 128:(t + 1) * 128, :], writes=[f_b])
        emit_post(P, f_t, f_b, x_t, x_b, g2, g2_b, s_t, s_b, junk, junk_b)
        outs.append(P.dma("gpsimd", d_out[t * 128:(t + 1) * 128, :], x_t[:], reads=[x_b]))
    end_phase(P, outs, nc)


SMALL = [("ada_bT", [128, 48]), ("ada_b", [6144]), ("npreT", [128, 8]), ("npost_mix", [1024]), ("npre_ffnT", [128, 8]),
         ("npost_ffn", [1024]), ("dlam", [4, 32]), ("subln", [64]), ("poolw", [2, 128, 128]), ("pscale", [128, 2]),
         ("sconvT", [128, 2, 3]), ("dconvT", [128, 6, 5]), ("alog", [8]), ("dtb", [8]), ("dnorm", [64]),
         ("bmergeT", [128, 4, 8])]
CORE_TABS = [("qaug", [2, 8, 3, 512], BF16), ("kaug", [4, 3, 4, TOK], BF16), ("biasF", [128, 4, 4, 32], F32),
             ("dtab", [128, 2, 4, 512], BF16), ("identb", [128, 128], BF16), ("ident", [128, 128], F32),
             ("bmask", [128, 2], F32), ("bcorr", [128, 2, 16], F32), ("binvw", [128, 2], F32),
             ("dconst", [64, 9, 64], F32)]


IDR = [("hT", [128, 8, TOK], BF16), ("qkT", [2, 128, TOK], BF16), ("kvloc", [NK + NV], BF16),
       ("kvall", [KVCH, 4, KVSZ], BF16), ("fmT", [12, 128, TOK], F32), ("dz", [TOK, 256], F32), ("dbg", [TOK, 16], F32),
       ("edge_loc", [10, 128, 16], F32), ("edge_all", [4, 10, 128, 16], F32), ("oaT", [2, 128, TOK], BF16),
       ("obT", [2, 128, TOK], BF16), ("ocT", [2, 128, TOK], BF16), ("dsend", [16, 4, 256, DW], F32),
       ("dall", [16, 4, 4, 256, DW], F32), ("dloc", [SEQ, DW], F32), ("do", [16, 2, 1024, 64], F32),
       ("doall", [4, 4, 4, 2, 1024, 64], F32), ("dloc_o", [4, 2, TOK, 64], F32), ("xmid", [TOK, D], F32),
       ("mT", [128, 8, TOK], BF16), ("h2T", [128, 8, TOK], BF16), ("acc", [TOK, D], F32), ("gwd", [128, NT, 8], F32)]
GROUPS = {
    1: dict(steps=(0, 1), load=(), dump=("hT", "fmT", "dz", "dbg", "edge_all", "oaT"), w=("ada_w", "w_in")),
    2: dict(steps=(2, 3), load=("fmT", "edge_all", "dbg"), dump=("obT", "ocT", "dloc_o"), w=()),
    3: dict(steps=(4, 5, 6, 7), load=("hT", "oaT", "obT", "ocT", "dloc_o", "dz"), dump=(),
            w=("ada_w", "w_merge", "w_branch", "w_o")),
}
WLAYER = {"ada_w": (1024, 6144), "w_in": (1024, NCOL), "w_merge": (4096, 1024), "w_branch": (1024, 1024),
          "w_o": (1024, 1024), "ffn_w_gate": (1024, DFF), "ffn_w_up": (1024, DFF), "ffn_w_down": (DFF, 1024),
          "moe_w_gate": (8192, DFF), "moe_w_up": (8192, DFF), "moe_w_down": (8 * DFF, 1024)}


def build_group(l, g):
    import math
    G = GROUPS[g]
    moe = (l == 1)
    nc = new_nc()
    wn = list(G["w"])
    if g == 3:
        wn += ["moe_w_gate", "moe_w_up", "moe_w_down"] if moe else ["ffn_w_gate", "ffn_w_up", "ffn_w_down"]
    W = {n: din(nc, n, list(WLAYER[n])) for n in wn}
    D_ = {}
    for name, shp, dt in CORE_TABS:
        D_[name] = din(nc, name, shp, dt)
    for name, shp, dt in IDR:
        D_[name] = nc.dram_tensor("i_" + name, list(shp), dt).ap()
    sm = {name: din(nc, f"{name}", shp) for name, shp in SMALL}
    if moe:
        sm["router_w"] = din(nc, "router_w", [1024, 8])
        sm["router_b"] = din(nc, "router_b", [8])
    d_cT = din(nc, "cT", [128, 8])
    d_x = din(nc, "x", [TOK, D]) if g in (1, 3) else None
    d_y = dout(nc, "y", [TOK, D]) if g == 3 else None
    if G["load"]:
        P = Prog(nc)
        outs = []
        for i, name in enumerate(G["load"]):
            src = nc.dram_tensor("in_" + name, list(D_[name].shape), D_[name].dtype, kind="ExternalInput").ap()
            outs.append(P.dma(("sync", "gpsimd")[i % 2], D_[name], src))
        end_phase(P, outs, nc)
    lam_init = 0.8 - 0.6 * math.exp(-0.3 * l)
    steps = {
        0: lambda: phase_A(nc, 0, d_x, d_cT, W, sm, D_),
        1: lambda: phase_ATT(nc, 0, sm, D_, lam_init),
        2: lambda: phase_B(nc, 0, sm, D_),
        3: lambda: phase_DELTA(nc, 0, D_),
        4: lambda: (phase_C1a(nc, 0, W, sm, D_), phase_C1b(nc, 0, d_x, d_cT, W, sm, D_)),
        5: lambda: phase_F1(nc, 0, d_cT, W, sm, D_, moe),
        6: lambda: phase_F2(nc, 0, W, D_, moe),
        7: lambda: phase_F3(nc, 0, d_cT, d_y, W, sm, D_),
    }
    for si in G["steps"]:
        steps[si]()
    if G["dump"]:
        P = Prog(nc)
        outs = []
        for i, name in enumerate(G["dump"]):
            src = D_[name]
            dst = nc.dram_tensor("out_" + name, list(src.shape), src.dtype, kind="ExternalOutput").ap()
            outs.append(P.dma(("sync", "gpsimd")[i % 2], dst, src))
        end_phase(P, outs, nc)
    if getattr(nc, "_gsem", None) is not None:
        nc._gsem["stack"].close()
    return nc


def host_layer_inputs(inp, l, g, x_cur, carry):
    f32 = np.float32
    G = GROUPS[g]
    moe = (l == 1)
    wl = {
        "ada_w": inp["ada_w"][l], "w_in": inp["w_in"][l], "w_merge": inp["w_merge"][l].reshape(4096, 1024),
        "w_branch": inp["w_branch"][l].reshape(1024, 1024), "w_o": inp["w_o"][l],
        "ffn_w_gate": inp["ffn_w_gate"][0], "ffn_w_up": inp["ffn_w_up"][0], "ffn_w_down": inp["ffn_w_down"][0],
        "moe_w_gate": inp["moe_w_gate"][0].reshape(8192, DFF), "moe_w_up": inp["moe_w_up"][0].reshape(8192, DFF),
        "moe_w_down": inp["moe_w_down"][0].reshape(8 * DFF, 1024),
    }
    wn = list(G["w"])
    if g == 3:
        wn += ["moe_w_gate", "moe_w_up", "moe_w_down"] if moe else ["ffn_w_gate", "ffn_w_up", "ffn_w_down"]
    dconst = delta_host_consts()["dconst"]
    maps = []
    for core in range(8):
        b, q = core // 4, core % 4
        m = {"cT": np.ascontiguousarray(inp["c"][b].reshape(8, 128).T)}
        if g in (1, 3):
            m["x"] = np.ascontiguousarray(x_cur[b, q * TOK:(q + 1) * TOK])
        for n in wn:
            m[n] = np.ascontiguousarray(wl[n], dtype=f32)
        at = att_host_tables(core)
        for k in ("qaug", "kaug", "biasF", "dtab", "identb"):
            m[k] = at[k]
        m["ident"] = np.eye(128, dtype=f32)
        m["dconst"] = dconst
        bt = b_host_tables(core, inp, l)
        m["bmask"], m["bcorr"], m["binvw"] = bt["bmask"], bt["bcorr"], bt["binvw"]
        sm = {
            "ada_bT": np.ascontiguousarray(inp["ada_b"][l].reshape(48, 128).T), "ada_b": inp["ada_b"][l],
            "npreT": np.ascontiguousarray(inp["norm_mix_pre"][l].reshape(8, 128).T),
            "npost_mix": inp["norm_mix_post"][l],
            "npre_ffnT": np.ascontiguousarray(inp["norm_ffn_pre"][l].reshape(8, 128).T),
            "npost_ffn": inp["norm_ffn_post"][l], "dlam": inp["diff_lambda"][l], "subln": inp["diff_subln"][l],
            "poolw": bt["poolw"], "pscale": bt["pscale"], "sconvT": bt["sconvT"], "dconvT": bt["dconvT"],
            "alog": bt["alog"], "dtb": bt["dtb"], "dnorm": inp["delta_norm"][l],
            "bmergeT": np.ascontiguousarray(inp["b_merge"][l].reshape(4, 8, 128).transpose(2, 0, 1)),
        }
        for k, v in sm.items():
            m[k] = np.ascontiguousarray(v, dtype=f32)
        if moe:
            m["router_w"] = np.ascontiguousarray(inp["router_w"][0])
            m["router_b"] = np.ascontiguousarray(inp["router_b"][0])
        for name in G["load"]:
            m["in_" + name] = carry[core][name]
        maps.append(m)
    return maps


_NC_CACHE = {}


def kernel(**inputs):
    inp = {k: np.asarray(v) for k, v in inputs.items()}
    x_cur = np.asarray(inp["x"], dtype=np.float32)
    for l in range(2):
        carry = [dict() for _ in range(8)]
        for g in (1, 2, 3):
            nc = build_group(l, g)
            names = set(a.memorylocations[0].name for a in nc.allocations
                        if hasattr(a, "memorylocations") and a.kind == "ExternalInput")
            maps = host_layer_inputs(inp, l, g, x_cur, carry)
            maps = [{k: v for k, v in m.items() if k in names} for m in maps]
            import sys
            print(f"[kernel] launching layer {l} group {g}", file=sys.stderr, flush=True)
            res = run_bass_kernel_spmd(nc, maps, core_ids=list(range(8)))
            for core in range(8):
                for k, v in res.results[core].items():
                    if k.startswith("out_"):
                        carry[core][k[4:]] = np.asarray(v)
            if g == 3:
                x_new = np.zeros((2, SEQ, D), np.float32)
                for core in range(8):
                    b, q = core // 4, core % 4
                    x_new[b, q * TOK:(q + 1) * TOK] = res.results[core]["y"]
                x_cur = x_new
    return x_cur
```
